# Optimizing a Trainium2 kernel written in Bass

```python
import math
import jax, jax.numpy as jnp
from jax import lax
import numpy as np

D_MODEL = 2048
BATCH = 8
SEQ = 2048
DEPTH = 2
DEC_BATCH = 2
DEC_SEQ = 4096
PAST_LEN = 128

MLA_HEADS = 16
Q_LORA = 512
KV_LORA = 512
QK_NOPE = 128
QK_ROPE = 64
V_HEAD = 128
ROPE_THETA = 10000.0
Q_BLOCK = 128
MLA_SCALE = (QK_NOPE + QK_ROPE) ** -0.5
SGU_CHUNK = 128
SGU_GROUPS = 4
SGU_WIDTH = D_MODEL
FNET_GROUPS = 4
FNET_WIDTH = D_MODEL
MEM_LEN = 256
X_HEADS = 4
X_HEAD_DIM = D_MODEL // X_HEADS
N_EXPERTS = 32
TOP_K = 4
D_FF = D_MODEL
SWIGLU_LIMIT = 7.0
SWIGLU_ALPHA = 1.702
N_BRANCH = 3
DEEPNORM_ALPHA = (2 * DEPTH) ** 0.25
DEEPNORM_BETA = (8 * DEPTH) ** -0.25
LN_EPS = 1e-5
RMS_EPS = 1e-6
OFF_Q = 0
OFF_KV = OFF_Q + Q_LORA
OFF_KR = OFF_KV + KV_LORA
OFF_SGU = OFF_KR + QK_ROPE
OFF_FNET = OFF_SGU + 2 * SGU_WIDTH
OFF_GATE = OFF_FNET + FNET_WIDTH
N_IN = OFF_GATE + N_BRANCH * D_MODEL

kernel_name = 'hybrid_mla_sgu_fnet_moe_encoder'


def layer_norm(x, g, b):
    xf = x.astype(jnp.float32)
    mu = jnp.mean(xf, axis=-1, keepdims=True)
    var = jnp.mean(jnp.square(xf - mu), axis=-1, keepdims=True)
    y = (xf - mu) * lax.rsqrt(var + LN_EPS) * g.astype(jnp.float32) + b.astype(jnp.float32)
    return y.astype(x.dtype)


def rms_norm(x, g):
    xf = x.astype(jnp.float32)
    y = xf * lax.rsqrt(jnp.mean(jnp.square(xf), axis=-1, keepdims=True) + RMS_EPS) * g.astype(jnp.float32)
    return y.astype(x.dtype)


def rope_tables(seq, dtype):
    inv = 1.0 / (ROPE_THETA ** (jnp.arange(0, QK_ROPE, 2, dtype=jnp.float32) / QK_ROPE))
    ang = jnp.arange(seq, dtype=jnp.float32)[:, None] * inv[None, :]
    return jnp.cos(ang).astype(dtype), jnp.sin(ang).astype(dtype)


def apply_rope(t, cos, sin):
    half = QK_ROPE // 2
    t1, t2 = t[..., :half], t[..., half:]
    return jnp.concatenate([t1 * cos - t2 * sin, t1 * sin + t2 * cos], axis=-1)


def mla_attention(q_nope, q_rope, k_nope, k_rope, v):
    B, S, H, _ = q_nope.shape
    nb = S // Q_BLOCK

    def to_blocks(t):
        return jnp.moveaxis(t.reshape((B, nb, Q_BLOCK) + t.shape[2:]), 1, 0)

    def one_block(blk):
        qn, qr = blk
        s = (jnp.einsum('bqhd,bkhd->bhqk', qn, k_nope, preferred_element_type=jnp.float32)
             + jnp.einsum('bqhr,bkr->bhqk', qr, k_rope, preferred_element_type=jnp.float32)) * MLA_SCALE
        p = jax.nn.softmax(s, axis=-1).astype(v.dtype)
        return jnp.einsum('bhqk,bkhd->bqhd', p, v)

    o = lax.map(one_block, (to_blocks(q_nope), to_blocks(q_rope)))
    return jnp.moveaxis(o, 0, 1).reshape(B, S, H * V_HEAD)


def mla_branch(z, q_norm, kv_norm, w_uq, w_ukv, w_o):
    B, S, _ = z.shape
    c_q = rms_norm(z[..., OFF_Q:OFF_KV], q_norm)
    c_kv = rms_norm(z[..., OFF_KV:OFF_KR], kv_norm)
    cos, sin = rope_tables(S, z.dtype)
    k_rope = apply_rope(z[..., OFF_KR:OFF_SGU], cos[None], sin[None])
    q = (c_q @ w_uq).reshape(B, S, MLA_HEADS, QK_NOPE + QK_ROPE)
    kv = (c_kv @ w_ukv).reshape(B, S, MLA_HEADS, QK_NOPE + V_HEAD)
    q_rope = apply_rope(q[..., QK_NOPE:], cos[None, :, None, :], sin[None, :, None, :])
    o = mla_attention(q[..., :QK_NOPE], q_rope, kv[..., :QK_NOPE], k_rope, kv[..., QK_NOPE:])
    return o @ w_o


def sgu_branch(z, ln_g, ln_b, ws, bs, w_o):
    B, S, _ = z.shape
    zs = jax.nn.gelu(z[..., OFF_SGU:OFF_FNET], approximate=False)
    u, v = zs[..., :SGU_WIDTH], zs[..., SGU_WIDTH:]
    v = layer_norm(v, ln_g, ln_b)
    vc = v.reshape(B, S // SGU_CHUNK, SGU_CHUNK, SGU_GROUPS, SGU_WIDTH // SGU_GROUPS)
    s = jnp.einsum('gpq,bnqgc->bnpgc', ws, vc) + bs.T[:, :, None]
    return (u * s.reshape(B, S, SGU_WIDTH)) @ w_o


def fnet_branch(z, w_o):
    B, S, _ = z.shape
    f = z[..., OFF_FNET:OFF_GATE].astype(jnp.float32).reshape(B, S, FNET_GROUPS, FNET_WIDTH // FNET_GROUPS)
    f = jnp.fft.fft2(f, axes=(1, 3), norm='ortho').real.astype(z.dtype).reshape(B, S, FNET_WIDTH)
    return f @ w_o


def cross_attention(x, mem, w_q, w_k, w_v, w_o):
    B, S, _ = x.shape
    M = mem.shape[1]
    q = (x @ w_q).reshape(B, S, X_HEADS, X_HEAD_DIM)
    k = (mem @ w_k).reshape(B, M, X_HEADS, X_HEAD_DIM)
    v = (mem @ w_v).reshape(B, M, X_HEADS, X_HEAD_DIM)
    s = jnp.einsum('bqhd,bkhd->bhqk', q, k, preferred_element_type=jnp.float32) * (X_HEAD_DIM ** -0.5)
    p = jax.nn.softmax(s, axis=-1).astype(v.dtype)
    o = jnp.einsum('bhqk,bkhd->bqhd', p, v).reshape(B, S, D_MODEL)
    return o @ w_o


def moe_ffn(x, w_router, b_router, w_gu, b_gu, w_down, b_down):
    B, S, D = x.shape
    h = x.reshape(B * S, D)
    logits = (h @ w_router + b_router).astype(jnp.float32)
    top_v, top_i = lax.top_k(logits, TOP_K)
    probs = jax.nn.softmax(top_v, axis=-1)
    combine = jnp.sum(jax.nn.one_hot(top_i, N_EXPERTS, dtype=jnp.float32) * probs[..., None], axis=1).astype(h.dtype)
    y = jnp.zeros_like(h)
    for e in range(N_EXPERTS):
        gu = h @ w_gu[e] + b_gu[e]
        gate = jnp.minimum(gu[:, :D_FF], SWIGLU_LIMIT)
        up = jnp.clip(gu[:, D_FF:], -SWIGLU_LIMIT, SWIGLU_LIMIT)
        act = (up + 1.0) * gate * jax.nn.sigmoid(SWIGLU_ALPHA * gate)
        y = y + combine[:, e:e + 1] * (act @ w_down[e] + b_down[e])
    return y.reshape(B, S, D)


def encoder_trunk(x, mem, w_in, b_gate, mla_q_norm, mla_kv_norm, w_uq, w_ukv, w_mla_o,
                  sgu_ln_g, sgu_ln_b, sgu_ws, sgu_bs, w_sgu_o, w_fnet_o, w_out, ln1_g, ln1_b,
                  w_cq, w_ck, w_cv, w_co, ln2_g, ln2_b,
                  w_router, b_router, w_gu, b_gu, w_down, b_down, ln3_g, ln3_b):
    B, S, D = x.shape
    for l in range(DEPTH):
        z = x @ w_in[l]
        y_a = mla_branch(z, mla_q_norm[l], mla_kv_norm[l], w_uq[l], w_ukv[l], w_mla_o[l])
        y_b = sgu_branch(z, sgu_ln_g[l], sgu_ln_b[l], sgu_ws[l], sgu_bs[l], w_sgu_o[l])
        y_c = fnet_branch(z, w_fnet_o[l])
        g = jax.nn.sigmoid(z[..., OFF_GATE:] + b_gate[l]).reshape(B, S, N_BRANCH, D)
        m = g[:, :, 0] * y_a + g[:, :, 1] * y_b + g[:, :, 2] * y_c
        x = layer_norm(DEEPNORM_ALPHA * x + m @ w_out[l], ln1_g[l], ln1_b[l])
        x = layer_norm(DEEPNORM_ALPHA * x + cross_attention(x, mem, w_cq[l], w_ck[l], w_cv[l], w_co[l]), ln2_g[l], ln2_b[l])
        x = layer_norm(DEEPNORM_ALPHA * x + moe_ffn(x, w_router[l], b_router[l], w_gu[l], b_gu[l], w_down[l], b_down[l]), ln3_g[l], ln3_b[l])
    return x


def setup_inputs(seed: int = 0) -> dict:
    key = jax.random.key(seed)
    ks = iter(jax.random.split(key, 40))
    L = DEPTH

    def nrm(shape, scale):
        return jax.random.normal(next(ks), shape, jnp.float32) * scale

    def gain(shape):
        return 1.0 + 0.01 * jax.random.normal(next(ks), shape, jnp.float32)

    return {
        'x_prompt': nrm((BATCH, SEQ, D_MODEL), 1.0),
        'x_sample': nrm((DEC_BATCH, DEC_SEQ, D_MODEL), 1.0),
        'mem_prompt': nrm((BATCH, MEM_LEN, D_MODEL), 1.0),
        'mem_sample': nrm((DEC_BATCH, MEM_LEN, D_MODEL), 1.0),
        'w_in': nrm((L, D_MODEL, N_IN), D_MODEL ** -0.5),
        'b_gate': nrm((L, N_BRANCH * D_MODEL), 0.01),
        'mla_q_norm': gain((L, Q_LORA)),
        'mla_kv_norm': gain((L, KV_LORA)),
        'w_uq': nrm((L, Q_LORA, MLA_HEADS * (QK_NOPE + QK_ROPE)), Q_LORA ** -0.5),
        'w_ukv': nrm((L, KV_LORA, MLA_HEADS * (QK_NOPE + V_HEAD)), KV_LORA ** -0.5),
        'w_mla_o': nrm((L, MLA_HEADS * V_HEAD, D_MODEL), (MLA_HEADS * V_HEAD) ** -0.5),
        'sgu_ln_g': gain((L, SGU_WIDTH)),
        'sgu_ln_b': nrm((L, SGU_WIDTH), 0.01),
        'sgu_ws': nrm((L, SGU_GROUPS, SGU_CHUNK, SGU_CHUNK), SGU_CHUNK ** -0.5),
        'sgu_bs': gain((L, SGU_GROUPS, SGU_CHUNK)),
        'w_sgu_o': nrm((L, SGU_WIDTH, D_MODEL), SGU_WIDTH ** -0.5),
        'w_fnet_o': nrm((L, FNET_WIDTH, D_MODEL), FNET_WIDTH ** -0.5),
        'w_out': nrm((L, D_MODEL, D_MODEL), DEEPNORM_BETA * D_MODEL ** -0.5),
        'ln1_g': gain((L, D_MODEL)),
        'ln1_b': nrm((L, D_MODEL), 0.01),
        'w_cq': nrm((L, D_MODEL, D_MODEL), D_MODEL ** -0.5),
        'w_ck': nrm((L, D_MODEL, D_MODEL), D_MODEL ** -0.5),
        'w_cv': nrm((L, D_MODEL, D_MODEL), D_MODEL ** -0.5),
        'w_co': nrm((L, D_MODEL, D_MODEL), DEEPNORM_BETA * D_MODEL ** -0.5),
        'ln2_g': gain((L, D_MODEL)),
        'ln2_b': nrm((L, D_MODEL), 0.01),
        'w_router': nrm((L, D_MODEL, N_EXPERTS), D_MODEL ** -0.5),
        'b_router': nrm((L, N_EXPERTS), 0.01),
        'w_gu': nrm((L, N_EXPERTS, D_MODEL, 2 * D_FF), D_MODEL ** -0.5),
        'b_gu': nrm((L, N_EXPERTS, 2 * D_FF), 0.01),
        'w_down': nrm((L, N_EXPERTS, D_FF, D_MODEL), DEEPNORM_BETA * D_FF ** -0.5),
        'b_down': nrm((L, N_EXPERTS, D_MODEL), 0.01),
        'ln3_g': gain((L, D_MODEL)),
        'ln3_b': nrm((L, D_MODEL), 0.01),
    }


def reference(x_prompt, x_sample, mem_prompt, mem_sample, w_in, b_gate, mla_q_norm, mla_kv_norm,
              w_uq, w_ukv, w_mla_o, sgu_ln_g, sgu_ln_b, sgu_ws, sgu_bs, w_sgu_o, w_fnet_o, w_out,
              ln1_g, ln1_b, w_cq, w_ck, w_cv, w_co, ln2_g, ln2_b,
              w_router, b_router, w_gu, b_gu, w_down, b_down, ln3_g, ln3_b):
    weights = (w_in, b_gate, mla_q_norm, mla_kv_norm, w_uq, w_ukv, w_mla_o,
               sgu_ln_g, sgu_ln_b, sgu_ws, sgu_bs, w_sgu_o, w_fnet_o, w_out, ln1_g, ln1_b,
               w_cq, w_ck, w_cv, w_co, ln2_g, ln2_b,
               w_router, b_router, w_gu, b_gu, w_down, b_down, ln3_g, ln3_b)
    y_prompt = encoder_trunk(x_prompt, mem_prompt, *weights)
    y_sample = encoder_trunk(x_sample, mem_sample, *weights)
    return (y_prompt, y_sample)
```

```python
import numpy as np
from contextlib import ExitStack
import concourse.bass as bass
import concourse.mybir as mybir
from concourse.bass_utils import run_bass_kernel_spmd

F32 = mybir.dt.float32
BF16 = mybir.dt.bfloat16
I32 = mybir.dt.int32
AF = mybir.ActivationFunctionType
ALU = mybir.AluOpType
AX = mybir.AxisListType

SEM_BIAS = 20000
USE_LOOPS = False


class LE:
    __slots__ = ("c", "t")

    def __init__(self, c=0, t=None):
        self.c = c
        self.t = t or {}

    def add(self, k):
        return LE(self.c + k, self.t)

    def addvar(self, v, coef):
        t = dict(self.t)
        t[v] = t.get(v, 0) + coef
        if t[v] == 0:
            del t[v]
        return LE(self.c, t)

    def subst(self, v, val):
        if v not in self.t:
            return self
        t = dict(self.t)
        coef = t.pop(v)
        return LE(self.c + coef * val, t)

    def same(self, o):
        return self.t == o.t


class Buf:
    def __init__(self, S, name):
        self.name = name
        self.ws = []
        self.rs = []
        S.bufs.append(self)


def _prune(toks):
    out = []
    for t in toks:
        k, le, ser = t
        rep = False
        for j, (k2, le2, ser2) in enumerate(out):
            if k2 == k and le2.same(le):
                if le.c > le2.c:
                    out[j] = t
                rep = True
                break
        if not rep:
            out.append(t)
    return out


class Sched:
    NDMASEM = 40

    def __init__(self, nc, es):
        self.nc = nc
        self.es = es
        self.eng = {"pe": nc.tensor, "act": nc.scalar, "dve": nc.vector, "pool": nc.gpsimd, "sp": nc.sync}
        self.sems = {}
        self.cnt = {}
        self.seen = {e: {} for e in self.eng}
        self.dry = False
        self.loopvars = {}
        self.nvars = 0
        self.bufs = []
        self.serial = 0
        self.pending = {e: [] for e in self.eng}
        self.dmakeys = {}
        self.free_dsems = []
        self.ninstr = 0
        for e in self.eng:
            self._mksem("E_" + e)
        for i in range(self.NDMASEM):
            k = "D_%d" % i
            self._mksem(k)
            self.free_dsems.append(k)
        for k, s in self.sems.items():
            left = SEM_BIAS
            while left > 0:
                step = min(left, 10000)
                nc.sync.sem_inc(s, step)
                left -= step
            self.cnt[k] = LE(SEM_BIAS)

    def _mksem(self, key):
        self.sems[key] = self.es.enter_context(self.nc.semaphore(key))
        self.cnt[key] = LE(0)

    def buf(self, name):
        return Buf(self, name)

    def bufs_n(self, name, n):
        return [Buf(self, "%s%d" % (name, i)) for i in range(n)]

    def val(self, le):
        v = le.c
        for var, coef in le.t.items():
            v = self.loopvars[var] * coef + v
        return v

    def _wait(self, e, tok):
        key, le, _ = tok
        if key == "E_pe" and e == "pe":
            return
        s = self.seen[e].get(key)
        if s is not None and s.same(le) and s.c >= le.c:
            return
        if not self.dry:
            self.eng[e].wait_ge(self.sems[key], self.val(le))
            self.ninstr += 1
        if s is None or not s.same(le) or s.c < le.c:
            self.seen[e][key] = le

    def _deps(self, reads, writes):
        deps = []
        for b in reads:
            deps += b.ws
        for b in writes:
            deps += b.ws
            deps += b.rs
        return deps

    def _commit(self, tok, reads, writes):
        for b in reads:
            b.rs = _prune(b.rs + [tok])
        for b in writes:
            b.ws = [tok]
            b.rs = []

    def op(self, e, fn, reads=(), writes=(), signal=True):
        for tok in self._deps(reads, writes):
            self._wait(e, tok)
        ins = None
        if not self.dry:
            ins = fn()
            self.ninstr += 1
        if signal:
            key = "E_" + e
            self.cnt[key] = self.cnt[key].add(1)
            if not self.dry:
                ins.then_inc(self.sems[key], 1)
            self.serial += 1
            tok = (key, self.cnt[key], self.serial)
            for (r, w) in self.pending[e]:
                self._commit(tok, r, w)
            self.pending[e] = []
            self._commit(tok, reads, writes)
        else:
            self.pending[e].append((tuple(reads), tuple(writes)))

    def dma(self, q, pairs, reads=(), writes=(), key=None, **kw):
        if key is None:
            key = "_anon_%s" % q
        if key not in self.dmakeys:
            sk = self.free_dsems.pop(0)
            self.dmakeys[key] = (sk, Buf(self, "dsem_" + key))
        sk, sbuf = self.dmakeys[key]
        for tok in self._deps(reads, tuple(writes) + (sbuf,)):
            self._wait(q, tok)
        if callable(pairs):
            pairs = pairs() if not self.dry else [None] * pairs.n
        n = len(pairs)
        if not self.dry:
            for (o, i) in pairs:
                self.eng[q].dma_start(out=o, in_=i, **kw).then_inc(self.sems[sk], 16)
                self.ninstr += 1
        self.cnt[sk] = self.cnt[sk].add(16 * n)
        self.serial += 1
        tok = (sk, self.cnt[sk], self.serial)
        self._commit(tok, reads, tuple(writes) + (sbuf,))

    def dma_raw(self, q, fn, reads=(), writes=(), key=None):
        if key not in self.dmakeys:
            sk = self.free_dsems.pop(0)
            self.dmakeys[key] = (sk, Buf(self, "dsem_" + key))
        sk, sbuf = self.dmakeys[key]
        for tok in self._deps(reads, tuple(writes) + (sbuf,)):
            self._wait(q, tok)
        if not self.dry:
            fn().then_inc(self.sems[sk], 16)
            self.ninstr += 1
        self.cnt[sk] = self.cnt[sk].add(16)
        self.serial += 1
        tok = (sk, self.cnt[sk], self.serial)
        self._commit(tok, reads, tuple(writes) + (sbuf,))

    def _snapshot(self):
        return (
            {id(b): (list(b.ws), list(b.rs)) for b in self.bufs},
            dict(self.cnt),
            {e: dict(m) for e, m in self.seen.items()},
            {e: list(p) for e, p in self.pending.items()},
            dict(self.dmakeys),
            list(self.free_dsems),
            len(self.bufs),
        )

    def _restore(self, snap):
        bs, cnt, seen, pend, dk, fd, nb = snap
        del self.bufs[nb:]
        for b in self.bufs:
            b.ws, b.rs = list(bs[id(b)][0]), list(bs[id(b)][1])
        self.cnt = dict(cnt)
        self.seen = {e: dict(m) for e, m in seen.items()}
        self.pending = {e: list(p) for e, p in pend.items()}
        self.dmakeys = dict(dk)
        self.free_dsems = list(fd)

    def loop(self, trips, body):
        if trips == 1:
            body(0)
            return
        for e in self.eng:
            assert not self.pending[e], "unsignalled ops pending at loop entry"
        snap = self._snapshot()
        serial0 = self.serial
        entry = dict(self.cnt)
        was_dry = self.dry
        self.dry = True
        body(0)
        for e in self.eng:
            assert not self.pending[e], "unsignalled ops pending at loop end"
        n = {k: self.cnt[k].c - entry[k].c for k in self.cnt}
        new_keys = dict(self.dmakeys)
        new_free = list(self.free_dsems)
        end = {id(b): (list(b.ws), list(b.rs)) for b in self.bufs}
        allbufs = list(self.bufs)
        self._restore(snap)
        self.bufs = allbufs
        self.dmakeys = new_keys
        self.free_dsems = new_free
        for b in self.bufs:
            if id(b) not in snap[0]:
                b.ws, b.rs = [], []
        self.dry = was_dry
        if self.dry:
            for k in self.cnt:
                self.cnt[k] = entry[k].add(trips * n[k])
            for b in self.bufs:
                ws, rs = end[id(b)]
                b.ws = [(k, le.add((trips - 1) * n[k]), s) if s > serial0 else (k, le, s) for (k, le, s) in ws]
                b.rs = [(k, le.add((trips - 1) * n[k]), s) if s > serial0 else (k, le, s) for (k, le, s) in rs]
            return
        vid = self.nvars
        self.nvars += 1
        with self.nc.Fori(0, trips) as iv:
            self.loopvars[vid] = iv
            for k in self.cnt:
                if n[k]:
                    self.cnt[k] = entry[k].addvar(vid, n[k])
            for b in self.bufs:
                ws, rs = end[id(b)]
                cw = [(k, le.add(-n[k]).addvar(vid, n[k]), s) for (k, le, s) in ws if s > serial0]
                cr = [(k, le.add(-n[k]).addvar(vid, n[k]), s) for (k, le, s) in rs if s > serial0]
                b.ws = _prune(b.ws + cw)
                b.rs = _prune(b.rs + cr)
            self.seen = {e: {} for e in self.eng}
            body(iv)
        del self.loopvars[vid]
        for k in self.cnt:
            self.cnt[k] = entry[k].add(trips * n[k])
        for b in self.bufs:
            b.ws = _prune([(k, le.subst(vid, trips - 1), s) for (k, le, s) in b.ws])
            b.rs = _prune([(k, le.subst(vid, trips - 1), s) for (k, le, s) in b.rs])
        self.seen = {e: {} for e in self.eng}

    def finish(self, e="sp"):
        for k in self.cnt:
            self._wait(e, (k, self.cnt[k], 0))

    def barrier(self):
        last = getattr(self, "_bar_cnt", {})
        for e in self.eng:
            for k in self.cnt:
                le = self.cnt[k]
                if k in last and last[k].same(le) and last[k].c == le.c:
                    continue
                self._wait(e, (k, le, 0))
        self._bar_cnt = dict(self.cnt)
        for key, (sk, b) in self.dmakeys.items():
            self.free_dsems.append(sk)
        self.dmakeys = {}


D = 2048
T = 4096
NTB = T // 512
NTT = T // 128
DEPTH = 2
H = 16
OFF_Q, OFF_KV, OFF_KR, OFF_U, OFF_V, OFF_F, OFF_G, N_IN = 0, 512, 1024, 1088, 3136, 5184, 7232, 13376
ALPHA = (2 * DEPTH) ** 0.25
MLA_SCALE = 192 ** -0.5
NEXP = 32
CAP = 768
MEM = 256


class Prog:
    def __init__(self, cfg=None):
        self.cfg = cfg or {}
        self.nc = bass.Bass("TRN2", target_bir_lowering=False)
        self.es = ExitStack()
        self.dram = {}
        self.expose_in = set(self.cfg.get("expose_in", ()))
        self.expose_out = set(self.cfg.get("expose_out", ()))

    def dt(self, name, shape, dtype, kind=None):
        if kind is None:
            kind = "Internal"
            if name in self.expose_in:
                kind = "ExternalInput"
            elif name in self.expose_out:
                kind = "ExternalOutput"
        t = self.nc.dram_tensor(name, list(shape), dtype, kind=kind).ap()
        self.dram[name] = (t, kind, tuple(shape), dtype)
        return t

    def sb(self, es, name, shape, dtype):
        self._nsb = getattr(self, "_nsb", 0) + 1
        return es.enter_context(self.nc.sbuf_tensor("%s_%d" % (name, self._nsb), list(shape), dtype))


def _ceil(a, b):
    return (a + b - 1) // b


class Gemm:
    def __init__(self, P, S, es, KC, pw=512, nslots=2, wname="wp"):
        self.P, self.S = P, S
        nc = P.nc
        self.KC, self.pw = KC, pw
        self.wp = [P.sb(es, "%s%d" % (wname, i), [128, KC, pw], BF16) for i in range(nslots)]
        self.b_wp = S.bufs_n(wname, nslots)
        self.nslots = nslots
        self.slot = 0

    def run(self, A, bA, W, N, orient, epi, Tc=T, w_is_f32=True, unroll=False, acols=None):
        P, S, nc = self.P, self.S, self.P.nc
        KC, pw = self.KC, self.pw
        wv = W.rearrange("(c p) n -> p c n", p=128)
        wq = "pool" if w_is_f32 else "sp"
        full, rem = N // pw, N % pw

        def panel(pi, width, slot):
            wp = self.wp[slot]
            S.dma(wq, [(wp[:, :, 0:width], wv[:, :, bass.ds(pi * pw, width)])], writes=[self.b_wp[slot]],
                  key="wp%d" % slot)
            if orient == "fm":
                for sub in range(_ceil(width, 128)):
                    m = min(128, width - sub * 128)
                    for tb in range(Tc // 512):
                        q = (sub * (Tc // 512) + tb) % P.NGPS
                        for k in range(KC):
                            S.op("pe", lambda k=k, q=q, tb=tb, m=m, sub=sub: nc.tensor.matmul(
                                P.ps[q][0:m, :], wp[:, k, sub * 128:sub * 128 + m], A[:, k, tb * 512:(tb + 1) * 512],
                                start=(k == 0), stop=(k == KC - 1)),
                                reads=[self.b_wp[slot], bA], writes=[P.b_ps[q]], signal=(k == KC - 1))
                        epi(q, m, pi * (pw // 128) + sub, tb, 512)
            else:
                for tt in range(Tc // 128):
                    q = tt % P.NGPS
                    for k in range(KC):
                        S.op("pe", lambda k=k, q=q, tt=tt: nc.tensor.matmul(
                            P.ps[q][:, 0:width], A[:, k, tt * 128:(tt + 1) * 128], wp[:, k, 0:width],
                            start=(k == 0), stop=(k == KC - 1)),
                            reads=[self.b_wp[slot], bA], writes=[P.b_ps[q]], signal=(k == KC - 1))
                    epi(q, 128, pi * pw, tt, width)

        ns = self.nslots
        if USE_LOOPS and full >= 2 * ns and not unroll and full % ns == 0:
            def body(i):
                for s in range(ns):
                    panel(i * ns + s, pw, s)
            S.loop(full // ns, body)
        else:
            for pi in range(full):
                panel(pi, pw, pi % ns)
        if rem:
            panel(full, rem, full % ns)


def build_program(cfg=None):
    cfg = cfg or {}
    P = Prog(cfg)
    nc = P.nc
    es = P.es
    S = Sched(nc, es)
    P.S = S
    layers = cfg.get("layers", list(range(DEPTH)))
    phases = cfg.get("phases", None)

    def want(name):
        return phases is None or name in phases

    EI = "ExternalInput"
    x_in = P.dt("x_in", [T, D], F32, EI)
    w_in = P.dt("w_in", [DEPTH, D, N_IN], F32, EI)
    b_gate = P.dt("b_gate", [DEPTH, 3 * D, 1], F32, EI)
    ident_d = P.dt("ident", [128, 128], BF16, EI)
    cT = P.dt("cT", [1088, T], F32)
    uT = P.dt("uT", [D, T], BF16)
    vtok = P.dt("vtok", [T, D], BF16)
    fT = P.dt("fT", [D, T], BF16)
    gT = P.dt("gT", [3 * D, T], BF16)

    P.NGPS = 4
    P.pst = es.enter_context(nc.psum_tensor("pst", [128, 8, 512], F32))
    P.ps = [P.pst[:, i, :] for i in range(8)]
    P.b_ps = S.bufs_n("ps", 8)
    ident = P.sb(es, "ident_sb", [128, 128], BF16)
    b_const = S.buf("const")
    S.dma("sp", [(ident[:], ident_d[:, :])], writes=[b_const], key="const")

    def stage_out(pes, name, n, shape, dtype):
        tiles = [P.sb(pes, "%s%d" % (name, i), shape, dtype) for i in range(n)]
        return tiles, S.bufs_n(name, n)


    def build_xT(xT, b_xT, xs, b_xs, x_src, ntok):
        for tt in range(ntok // 128):
            s = tt % 2
            xrow = xs[s][:, 0:4, :].rearrange("p a b -> p (a b)")
            S.dma("pool", [(xrow, x_src[tt * 128:(tt + 1) * 128, :])], writes=[b_xs[s]], key="wp%d" % s)
            for half in range(2):
                q = 4 + (tt * 2 + half) % 2
                pst = P.ps[q].bitcast(BF16)
                for j in range(8):
                    c = half * 8 + j
                    S.op("pe", lambda c=c, j=j, pst=pst, xrow=xrow: nc.tensor.transpose(
                        pst[:, j * 128:(j + 1) * 128], xrow[:, c * 128:(c + 1) * 128], ident[:]),
                        reads=[b_xs[s], b_const], writes=[P.b_ps[q]], signal=(j == 7))
                dst = xT[:, half * 8:(half + 1) * 8, tt * 128:(tt + 1) * 128]
                src = pst.rearrange("p (j t) -> p j t", j=8)
                if half == 0:
                    S.op("act", lambda dst=dst, src=src: nc.scalar.activation(out=dst, in_=src, func=AF.Copy),
                         reads=[P.b_ps[q]], writes=[b_xT])
                else:
                    S.op("dve", lambda dst=dst, src=src: nc.vector.tensor_copy(out=dst, in_=src),
                         reads=[P.b_ps[q]], writes=[b_xT])

    def phase_zgemm(l, x_src):
        with ExitStack() as pes:
            xT = P.sb(pes, "xT", [128, 16, T], BF16)
            b_xT = S.buf("xT")
            G = Gemm(P, S, pes, 16)
            build_xT(xT, b_xT, G.wp, G.b_wp, x_src, T)
            o32, b_o32 = stage_out(pes, "o32_", 2, [128, 512], F32)
            o16, b_o16 = stage_out(pes, "o16_", 4, [128, 512], BF16)
            bg, b_bg = stage_out(pes, "bg_", 2, [128, 1], F32)
            st = {"i": 0}

            def epi_lat(q, m, n0, tb, width):
                j = (tb) % 2
                S.op("act", lambda: nc.scalar.activation(out=o32[j][0:m, :], in_=P.ps[q][0:m, :], func=AF.Copy),
                     reads=[P.b_ps[q]], writes=[b_o32[j]])
                S.dma("sp", [(cT[n0 * 128:n0 * 128 + m, tb * 512:(tb + 1) * 512], o32[j][0:m, :])], reads=[b_o32[j]],
                      key="st32_%d" % j)

            def mk_epi_fm(dst, func, bias_src=None):
                def epi(q, m, n0, tb, width):
                    j = tb % 4
                    if bias_src is not None and tb == 0:
                        S.dma("sp", [(bg[0][:, :], bias_src.rearrange("(j p) o -> j p o", p=128)[n0])], writes=[b_bg[0]], key="bg")
                    if bias_src is not None:
                        S.op("act", lambda: nc.scalar.activation(out=o16[j][:, :], in_=P.ps[q][:, :], func=func,
                                                                 bias=bg[0][:, 0:1]),
                             reads=[P.b_ps[q], b_bg[0]], writes=[b_o16[j]])
                    else:
                        S.op("act", lambda: nc.scalar.activation(out=o16[j][:, :], in_=P.ps[q][:, :], func=func),
                             reads=[P.b_ps[q]], writes=[b_o16[j]])
                    S.dma("sp", [(dst.rearrange("(j p) t -> j p t", p=128)[n0][:, tb * 512:(tb + 1) * 512], o16[j][:, :])], reads=[b_o16[j]],
                          key="st16_%d" % j)
                return epi

            def epi_v(q, m, n0, tt, width):
                j = tt % 4
                S.op("act", lambda: nc.scalar.activation(out=o16[j][:, :], in_=P.ps[q][:, :], func=AF.Gelu),
                     reads=[P.b_ps[q]], writes=[b_o16[j]])
                S.dma("sp", [(vtok[tt * 128:(tt + 1) * 128, bass.ds(n0, 512)], o16[j][:, :])], reads=[b_o16[j]],
                      key="st16_%d" % j)

            W = w_in[l]
            G.run(xT, b_xT, W[:, 0:OFF_U], OFF_U, "fm", epi_lat)
            G.run(xT, b_xT, W[:, OFF_U:OFF_V], D, "fm", mk_epi_fm(uT, AF.Gelu))
            G.run(xT, b_xT, W[:, OFF_V:OFF_F], D, "tm", epi_v)
            G.run(xT, b_xT, W[:, OFF_F:OFF_G], D, "fm", mk_epi_fm(fT, AF.Copy))
            G.run(xT, b_xT, W[:, OFF_G:N_IN], 3 * D, "fm", mk_epi_fm(gT, AF.Sigmoid, bias_src=b_gate[l]))
            S.barrier()


    qnorm_d = P.dt("mla_q_norm", [DEPTH, 128, 4], F32, EI)
    kvnorm_d = P.dt("mla_kv_norm", [DEPTH, 128, 4], F32, EI)
    w_uq = P.dt("w_uq", [DEPTH, 512, 3072], F32, EI)
    w_ukv = P.dt("w_ukv", [DEPTH, 512, 4096], F32, EI)
    rope_cs = P.dt("rope_cs", [2, 64, T], F32, EI)
    qT = P.dt("qT", [H, 192, T], BF16)
    kT = P.dt("kT", [H, 128, T], BF16)
    krT = P.dt("krT", [64, T], BF16)
    vtm = P.dt("vtm", [T, D], BF16)
    ones = P.sb(es, "ones_sb", [128, 128], BF16)
    S.op("dve", lambda: nc.vector.memset(ones[:], 1.0), writes=[b_const])

    def evac(i, out, in_, reads, writes, func=None, **kw):
        if i % 2 == 0:
            S.op("act", lambda: nc.scalar.activation(out=out, in_=in_, func=AF.Copy), reads=reads, writes=writes)
        else:
            S.op("dve", lambda: nc.vector.tensor_copy(out=out, in_=in_), reads=reads, writes=writes)

    def phase_mla_prep(l):
        with ExitStack() as pes:
            wq = P.sb(pes, "wq_all", [128, 4, 3072], BF16)
            wqs = P.sb(pes, "wq_s", [128, 4, 16, 64], BF16)
            wkc = P.sb(pes, "wk_c", [128, 4, 2048], BF16)
            wvc = P.sb(pes, "wv_c", [128, 4, 2048], BF16)
            gq = P.sb(pes, "gq", [128, 8], F32)
            b_w = S.buf("mlaw")
            wkv_v = w_ukv[l].rearrange("(c p) (h e) -> p c h e", p=128, e=256)
            S.dma("pool", [(wq[:], w_uq[l].rearrange("(c p) n -> p c n", p=128))]
                  + [(wkc[:, c, :].rearrange("p (h e) -> p h e", e=128), wkv_v[:, c, :, 0:128]) for c in range(4)]
                  + [(wvc[:, c, :].rearrange("p (h e) -> p h e", e=128), wkv_v[:, c, :, 128:256]) for c in range(4)],
                  writes=[b_w], key="mlaw")
            S.dma("sp", [(gq[:, 0:4], qnorm_d[l]), (gq[:, 4:8], kvnorm_d[l])], writes=[b_w], key="mlag")
            wq4 = wq[:].rearrange("p c (h e) -> p c h e", e=192)
            S.op("dve", lambda: nc.vector.tensor_copy(out=wqs[:, :, :, 0:32], in_=wq4[:, :, :, 160:192]), reads=[b_w], writes=[b_w])
            S.op("dve", lambda: nc.vector.tensor_copy(out=wqs[:, :, :, 32:64], in_=wq4[:, :, :, 128:160]), reads=[b_w], writes=[b_w])
            NB = 2
            cb = [P.sb(pes, "cb%d" % i, [128, 9, 512], F32) for i in range(NB)]
            cbs = [P.sb(pes, "cbs%d" % i, [64, 512], F32) for i in range(NB)]
            cs = [P.sb(pes, "cs%d" % i, [64, 2, 512], F32) for i in range(NB)]
            b_cb = S.bufs_n("cb", NB)
            sq = P.sb(pes, "sq", [128, 8, 512], BF16)
            b_sq = S.buf("sq")
            rs = P.sb(pes, "rs", [128, 2, 512], F32)
            b_rs = S.buf("rs")
            cn = P.sb(pes, "cn", [128, 8, 512], BF16)
            b_cn = S.buf("cn")
            st_n = [P.sb(pes, "st_n%d" % i, [128, 16, 512], BF16) for i in range(2)]
            b_stn = S.bufs_n("stn", 2)
            st_r = P.sb(pes, "st_r", [64, 16, 512], BF16)
            b_str = S.buf("str")
            st_v = P.sb(pes, "st_v", [128, 4, 2048], BF16)
            b_stv = S.buf("stv")
            st_kr = P.sb(pes, "st_kr", [64, 512], BF16)
            b_stkr = S.buf("stkr")
            t1 = [P.sb(pes, "t1_%d" % i, [64, 512], F32) for i in range(2)]
            t2 = [P.sb(pes, "t2_%d" % i, [64, 512], F32) for i in range(2)]
            b_t = S.bufs_n("t12", 2)
            cv = cT.rearrange("(c p) t -> p c t", p=128) if False else None
            for tb in range(NTB):
                s = tb % NB
                tsl = slice(tb * 512, (tb + 1) * 512)
                S.dma("sp", [(cb[s][:, 0:8, :], cT[0:1024, tsl].rearrange("(c p) t -> p c t", p=128)),
                             (cb[s][0:64, 8, :], cT[1024:1088, tsl]),
                             (cbs[s][0:32, :], cT[1056:1088, tsl]),
                             (cbs[s][32:64, :], cT[1024:1056, tsl]),
                             (cs[s][:, 0, :], rope_cs[0][:, tsl]),
                             (cs[s][:, 1, :], rope_cs[1][:, tsl])], writes=[b_cb[s]], key="cb%d" % s)
                S.op("act", lambda s=s: nc.scalar.activation(out=sq[:], in_=cb[s][:, 0:8, :], func=AF.Square),
                     reads=[b_cb[s]], writes=[b_sq])
                for g in range(2):
                    q = 6 + g
                    for k in range(4):
                        S.op("pe", lambda g=g, k=k, q=q: nc.tensor.matmul(P.ps[q][:], ones[:], sq[:, g * 4 + k, :], start=(k == 0), stop=(k == 3)),
                             reads=[b_sq, b_const], writes=[P.b_ps[q]], signal=(k == 3))
                    S.op("act", lambda g=g, q=q: nc.scalar.activation(out=rs[:, g, :], in_=P.ps[q][:], func=AF.Sqrt, scale=1.0 / 512, bias=1e-6),
                         reads=[P.b_ps[q]], writes=[b_rs])
                S.op("dve", lambda: nc.vector.reciprocal(out=rs[:], in_=rs[:]), reads=[b_rs], writes=[b_rs])
                for k in range(8):
                    S.op("dve", lambda k=k, s=s: nc.vector.scalar_tensor_tensor(out=cn[:, k, :], in0=cb[s][:, k, :], scalar=gq[:, k:k + 1],
                                                                          in1=rs[:, k // 4, :], op0=ALU.mult, op1=ALU.mult),
                         reads=[b_cb[s], b_rs, b_w], writes=[b_cn])
                sn = st_n[0]
                for h in range(H):
                    q = h % 2
                    for k in range(4):
                        S.op("pe", lambda h=h, k=k, q=q: nc.tensor.matmul(P.ps[q][:], wq[:, k, h * 192:h * 192 + 128], cn[:, k, :], start=(k == 0), stop=(k == 3)),
                             reads=[b_w, b_cn], writes=[P.b_ps[q]], signal=(k == 3))
                    evac(h, sn[:, h, :], P.ps[q][:], [P.b_ps[q]], [b_stn[0]])
                    qa, qb = 2 + (h % 2) * 2, 3 + (h % 2) * 2
                    for k in range(4):
                        S.op("pe", lambda h=h, k=k, qa=qa: nc.tensor.matmul(P.ps[qa][0:64, :], wq[:, k, h * 192 + 128:h * 192 + 192], cn[:, k, :], start=(k == 0), stop=(k == 3)),
                             reads=[b_w, b_cn], writes=[P.b_ps[qa]], signal=(k == 3))
                    for k in range(4):
                        S.op("pe", lambda h=h, k=k, qb=qb: nc.tensor.matmul(P.ps[qb][0:64, :], wqs[:, k, h, :], cn[:, k, :], start=(k == 0), stop=(k == 3)),
                             reads=[b_w, b_cn], writes=[P.b_ps[qb]], signal=(k == 3))
                    j = h % 2
                    S.op("dve", lambda j=j, qa=qa, s=s: nc.vector.tensor_tensor(out=t1[j][:], in0=P.ps[qa][0:64, :], in1=cs[s][:, 0, :], op=ALU.mult),
                         reads=[P.b_ps[qa], b_cb[s]], writes=[b_t[j]])
                    S.op("dve", lambda j=j, qb=qb, s=s: nc.vector.tensor_tensor(out=t2[j][:], in0=P.ps[qb][0:64, :], in1=cs[s][:, 1, :], op=ALU.mult),
                         reads=[P.b_ps[qb], b_cb[s]], writes=[b_t[j]])
                    S.op("dve", lambda j=j, h=h: nc.vector.tensor_tensor(out=st_r[:, h, :], in0=t1[j][:], in1=t2[j][:], op=ALU.add),
                         reads=[b_t[j]], writes=[b_str])
                S.dma("sp", [(qT[:, 0:128, tsl].rearrange("h p t -> p h t"), sn[:]),
                             (qT[:, 128:192, tsl].rearrange("h p t -> p h t"), st_r[:])], reads=[b_stn[0], b_str], key="stq")
                sk = st_n[1]
                for h in range(H):
                    q = h % 2
                    for k in range(4):
                        S.op("pe", lambda h=h, k=k, q=q: nc.tensor.matmul(P.ps[q][:], wkc[:, k, h * 128:h * 128 + 128], cn[:, 4 + k, :], start=(k == 0), stop=(k == 3)),
                             reads=[b_w, b_cn], writes=[P.b_ps[q]], signal=(k == 3))
                    evac(h + 1, sk[:, h, :], P.ps[q][:], [P.b_ps[q]], [b_stn[1]])
                S.dma("sp", [(kT[:, :, tsl].rearrange("h p t -> p h t"), sk[:])], reads=[b_stn[1]], key="stk")
                for tt in range(4):
                    for cp in range(4):
                        q = 2 + (tt * 4 + cp) % 4
                        for k in range(4):
                            S.op("pe", lambda tt=tt, cp=cp, k=k, q=q: nc.tensor.matmul(P.ps[q][:], cn[:, 4 + k, tt * 128:(tt + 1) * 128], wvc[:, k, cp * 512:(cp + 1) * 512],
                                                                                 start=(k == 0), stop=(k == 3)),
                                 reads=[b_w, b_cn], writes=[P.b_ps[q]], signal=(k == 3))
                        evac(tt * 4 + cp, st_v[:, tt, cp * 512:(cp + 1) * 512], P.ps[q][:], [P.b_ps[q]], [b_stv])
                S.dma("sp", [(vtm[tsl, :].rearrange("(a p) n -> p a n", p=128), st_v[:])], reads=[b_stv], key="stv")
                S.op("dve", lambda s=s: nc.vector.tensor_tensor(out=t1[0][:], in0=cb[s][0:64, 8, :], in1=cs[s][:, 0, :], op=ALU.mult),
                     reads=[b_cb[s]], writes=[b_t[0]])
                S.op("dve", lambda s=s: nc.vector.tensor_tensor(out=t2[0][:], in0=cbs[s][:], in1=cs[s][:, 1, :], op=ALU.mult),
                     reads=[b_cb[s]], writes=[b_t[0]])
                S.op("dve", lambda: nc.vector.tensor_tensor(out=st_kr[:], in0=t1[0][:], in1=t2[0][:], op=ALU.add),
                     reads=[b_t[0]], writes=[b_stkr])
                S.dma("sp", [(krT[:, tsl], st_kr[:])], reads=[b_stkr], key="stkr")
            S.barrier()


    amask_d = P.dt("attn_mask", [128, NTT * NTB], F32, EI)
    oT = P.dt("oT", [D, T], BF16)

    def phase_attn(l):
        with ExitStack() as pes:
            NB = 2
            QA = [P.sb(pes, "QA%d" % i, [128, T], BF16) for i in range(NB)]
            QB = [P.sb(pes, "QB%d" % i, [128, T], BF16) for i in range(NB)]
            KA = [P.sb(pes, "KA%d" % i, [128, T], BF16) for i in range(NB)]
            V = [P.sb(pes, "V%d" % i, [128, NTT, 128], BF16) for i in range(NB)]
            b_hd = S.bufs_n("hd", NB)
            KB = P.sb(pes, "KB", [128, T], BF16)
            b_kb = S.buf("KB")
            sqA = P.sb(pes, "sqA", [128, T], BF16)
            sqB = P.sb(pes, "sqB", [128, T], BF16)
            b_sq = S.buf("asq")
            mask = P.sb(pes, "amask", [128, NTT * NTB], F32)
            biash = P.sb(pes, "biash", [128, NTT * NTB], F32)
            b_bias = S.buf("biash")
            mx = P.sb(pes, "mx", [128, 32], F32)
            b_mx = S.buf("mx")
            Pt = [P.sb(pes, "Pt%d" % i, [128, 2, 512], BF16) for i in range(4)]
            b_pt = S.bufs_n("Pt", 4)
            b_pp = S.bufs_n("pspair", 3)
            acc = [P.sb(pes, "acc%d" % i, [128, 2, 512], F32) for i in range(2)]
            accp = [P.sb(pes, "accp%d" % i, [128, 2, 512], F32) for i in range(2)]
            accb = [P.sb(pes, "accb%d" % i, [128, 2, 512], BF16) for i in range(2)]
            b_acc = S.bufs_n("acc", 2)
            b_accp = S.bufs_n("accp", 2)
            b_accb = S.bufs_n("accb", 2)
            rinv = [P.sb(pes, "rinv%d" % i, [128, 512], F32) for i in range(2)]
            b_rinv = S.bufs_n("rinv", 2)
            osb = [P.sb(pes, "osb%d" % i, [128, T], BF16) for i in range(2)]
            b_osb = S.bufs_n("osb", 2)
            S.op("dve", lambda: nc.vector.memset(KB[64:128, :], 0.0), writes=[b_kb])
            for i in range(NB):
                S.op("dve", lambda i=i: nc.vector.memset(QB[i][64:128, :], 0.0), writes=[b_hd[i]])
            S.dma("sp", [(KB[0:64, :], krT[:, :]), (mask[:], amask_d[:, :])], writes=[b_kb], key="kb")
            S.op("act", lambda: nc.scalar.activation(out=sqB[:], in_=KB[:], func=AF.Square), reads=[b_kb], writes=[b_sq])
            for c in range(NTB):
                q = 6 + c % 2
                S.op("pe", lambda c=c, q=q: nc.tensor.matmul(P.ps[q][:], ones[:], sqB[:, c * 512:(c + 1) * 512], start=True, stop=True),
                     reads=[b_sq, b_const], writes=[P.b_ps[q]])
                S.op("dve", lambda c=c, q=q: nc.vector.reduce_max(out=mx[:, 8 + c:9 + c], in_=P.ps[q][:], axis=AX.X), reads=[P.b_ps[q]], writes=[b_mx])
            S.op("dve", lambda: nc.vector.reduce_max(out=mx[:, 24:25], in_=mx[:, 8:16], axis=AX.X), reads=[b_mx], writes=[b_mx])

            def load_head(h):
                s = h % NB
                vv = vtm[:, h * 128:(h + 1) * 128].rearrange("(a p) d -> p a d", p=128)
                S.dma("sp", [(QA[s][:], qT[h][0:128, :]), (QB[s][0:64, :], qT[h][128:192, :]), (KA[s][:], kT[h])]
                      + [(V[s][:, a * 8:(a + 1) * 8, :], vv[:, a * 8:(a + 1) * 8, :]) for a in range(4)],
                      writes=[b_hd[s]], key="hd%d" % s)

            load_head(0)
            for h in range(H):
                s = h % NB
                if h + 1 < H:
                    load_head(h + 1)
                S.op("act", lambda s=s: nc.scalar.activation(out=sqA[:], in_=QA[s][:], func=AF.Square), reads=[b_hd[s]], writes=[b_sq])
                S.op("act", lambda s=s: nc.scalar.activation(out=sqB[:], in_=QB[s][:], func=AF.Square), reads=[b_hd[s]], writes=[b_sq])
                for c in range(NTB):
                    q = 6 + c % 2
                    S.op("pe", lambda c=c, q=q: nc.tensor.matmul(P.ps[q][:], ones[:], sqA[:, c * 512:(c + 1) * 512], start=True, stop=False),
                         reads=[b_sq, b_const], writes=[P.b_ps[q]], signal=False)
                    S.op("pe", lambda c=c, q=q: nc.tensor.matmul(P.ps[q][:], ones[:], sqB[:, c * 512:(c + 1) * 512], start=False, stop=True),
                         reads=[b_sq, b_const], writes=[P.b_ps[q]])
                    S.op("dve", lambda c=c, q=q: nc.vector.reduce_max(out=mx[:, c:c + 1], in_=P.ps[q][:], axis=AX.X), reads=[P.b_ps[q]], writes=[b_mx])
                S.op("act", lambda s=s: nc.scalar.activation(out=sqA[:], in_=KA[s][:], func=AF.Square), reads=[b_hd[s], b_sq], writes=[b_sq])
                for c in range(NTB):
                    q = 6 + c % 2
                    S.op("pe", lambda c=c, q=q: nc.tensor.matmul(P.ps[q][:], ones[:], sqA[:, c * 512:(c + 1) * 512], start=True, stop=True),
                         reads=[b_sq, b_const], writes=[P.b_ps[q]])
                    S.op("dve", lambda c=c, q=q: nc.vector.reduce_max(out=mx[:, 8 + c:9 + c], in_=P.ps[q][:], axis=AX.X), reads=[P.b_ps[q]], writes=[b_mx])
                S.op("dve", lambda: nc.vector.reduce_max(out=mx[:, 16:17], in_=mx[:, 0:8], axis=AX.X), reads=[b_mx], writes=[b_mx])
                S.op("dve", lambda: nc.vector.reduce_max(out=mx[:, 17:18], in_=mx[:, 8:16], axis=AX.X), reads=[b_mx], writes=[b_mx])
                S.op("dve", lambda: nc.vector.tensor_tensor(out=mx[:, 17:18], in0=mx[:, 17:18], in1=mx[:, 24:25], op=ALU.add), reads=[b_mx], writes=[b_mx])
                S.op("dve", lambda: nc.vector.tensor_tensor(out=mx[:, 18:19], in0=mx[:, 16:17], in1=mx[:, 17:18], op=ALU.mult), reads=[b_mx], writes=[b_mx])
                S.op("act", lambda: nc.scalar.activation(out=mx[:, 19:20], in_=mx[:, 18:19], func=AF.Sqrt, scale=MLA_SCALE * MLA_SCALE), reads=[b_mx], writes=[b_mx])
                S.op("dve", lambda: nc.vector.tensor_scalar(out=biash[:], in0=mask[:], scalar1=mx[:, 19:20], scalar2=None, op0=ALU.subtract),
                     reads=[b_mx, b_kb], writes=[b_bias])
                for qc in range(NTB):
                    j = qc % 2
                    qo = 6 + j
                    qs = slice(qc * 512, (qc + 1) * 512)
                    def unit_s(kp):
                        pq = kp % 3
                        pj = kp % 4
                        for t in range(2):
                            kt = kp * 2 + t
                            ks = slice(kt * 128, (kt + 1) * 128)
                            S.op("pe", lambda t=t, ks=ks: nc.tensor.matmul(P.pst[:, pq * 2 + t, :], KA[s][:, ks], QA[s][:, qs], start=True, stop=False),
                                 reads=[b_hd[s]], writes=[b_pp[pq]], signal=False)
                            S.op("pe", lambda t=t, ks=ks: nc.tensor.matmul(P.pst[:, pq * 2 + t, :], KB[:, ks], QB[s][:, qs], start=False, stop=True),
                                 reads=[b_hd[s], b_kb], writes=[b_pp[pq]], signal=(t == 1))
                        bcol = kp * 2 * NTB + qc
                        S.op("act", lambda: nc.scalar.activation(out=Pt[pj][:], in_=P.pst[:, pq * 2:pq * 2 + 2, :], func=AF.Exp, scale=MLA_SCALE,
                                                                 bias=biash[:, bcol:bcol + 1]),
                             reads=[b_pp[pq], b_bias], writes=[b_pt[pj]])

                    def unit_pv(kp):
                        pj = kp % 4
                        for t in range(2):
                            kt = kp * 2 + t
                            S.op("pe", lambda t=t, kt=kt: nc.tensor.matmul(P.ps[qo], V[s][:, kt, :], Pt[pj][:, t, :], start=(kt == 0), stop=(kt == NTT - 1)),
                                 reads=[b_hd[s], b_pt[pj]], writes=[P.b_ps[qo]], signal=(t == 1))
                        if kp % 2 == 0:
                            a, ba = acc[j], b_acc[j]
                        else:
                            a, ba = accp[j], b_accp[j]
                        if kp < 2:
                            S.op("dve", lambda: nc.vector.tensor_copy(out=a[:], in_=Pt[pj][:]), reads=[b_pt[pj]], writes=[ba])
                        else:
                            S.op("dve", lambda: nc.vector.tensor_tensor(out=a[:], in0=a[:], in1=Pt[pj][:], op=ALU.add), reads=[b_pt[pj]], writes=[ba])

                    LAG = 2
                    NKP = NTT // 2
                    for kp in range(NKP + LAG):
                        if kp < NKP:
                            unit_s(kp)
                        if kp >= LAG:
                            unit_pv(kp - LAG)
                    S.op("dve", lambda: nc.vector.tensor_tensor(out=accb[j][:], in0=acc[j][:], in1=accp[j][:], op=ALU.add),
                         reads=[b_acc[j], b_accp[j]], writes=[b_accb[j]])
                    S.op("pe", lambda: nc.tensor.matmul(P.ps[0], ones[:], accb[j][:, 0, :], start=True, stop=False),
                         reads=[b_accb[j], b_const], writes=[b_pp[0]], signal=False)
                    S.op("pe", lambda: nc.tensor.matmul(P.ps[0], ones[:], accb[j][:, 1, :], start=False, stop=True),
                         reads=[b_accb[j], b_const], writes=[b_pp[0]])
                    S.op("dve", lambda: nc.vector.reciprocal(out=rinv[j][:], in_=P.ps[0]), reads=[b_pp[0]], writes=[b_rinv[j]])
                    S.op("dve", lambda j=j, qo=qo, s=s, qs=qs: nc.vector.tensor_tensor(out=osb[s][:, qs], in0=P.ps[qo], in1=rinv[j][:], op=ALU.mult),
                         reads=[P.b_ps[qo], b_rinv[j]], writes=[b_osb[s]])
                S.dma("sp", [(oT[h * 128:(h + 1) * 128, :], osb[s][:])], reads=[b_osb[s]], key="osb%d" % s)
            S.barrier()


    sgu_g_d = P.dt("sgu_ln_g", [DEPTH, D], F32, EI)
    sgu_b_d = P.dt("sgu_ln_b", [DEPTH, D], F32, EI)
    sgu_ws_d = P.dt("sgu_ws", [DEPTH, 4, 128, 128], F32, EI)
    sgu_bs_d = P.dt("sgu_bs", [DEPTH, 512], F32, EI)
    dft_cc = P.dt("dft_cc", [2, 512, 512], BF16, EI)
    dft_s = P.dt("dft_s", [2, T, T], BF16, EI)
    sguT = P.dt("sguT", [D, T], BF16)
    Gcs = P.dt("Gcs", [2, T, D], BF16)
    fnT = P.dt("fnT", [D, T], BF16)

    def ln_rows(pes_tiles, h, out, gb, b_h, b_out, b_gb):
        st6, mv, b_small = pes_tiles
        for c in range(4):
            S.op("dve", lambda c=c: nc.vector.bn_stats(out=st6[:, c, :], in_=h[:, c * 512:(c + 1) * 512]), reads=[b_h], writes=[b_small])
        S.op("dve", lambda: nc.vector.bn_aggr(out=mv[:, 0:2], in_=st6[:]), reads=[b_small], writes=[b_small])
        S.op("act", lambda: nc.scalar.activation(out=mv[:, 2:3], in_=mv[:, 1:2], func=AF.Sqrt, bias=eps_t[:, 0:1]), reads=[b_small, b_const], writes=[b_small])
        S.op("dve", lambda: nc.vector.reciprocal(out=mv[:, 2:3], in_=mv[:, 2:3]), reads=[b_small], writes=[b_small])
        S.op("dve", lambda: nc.vector.scalar_tensor_tensor(out=mv[:, 3:4], in0=mv[:, 0:1], scalar=-1.0, in1=mv[:, 2:3], op0=ALU.mult, op1=ALU.mult),
             reads=[b_small], writes=[b_small])
        S.op("act", lambda: nc.scalar.activation(out=h[:], in_=h[:], func=AF.Identity, scale=mv[:, 2:3], bias=mv[:, 3:4]), reads=[b_small], writes=[b_h])
        S.op("dve", lambda: nc.vector.tensor_tensor(out=h[:], in0=h[:], in1=gb[0][:], op=ALU.mult), reads=[b_gb], writes=[b_h])
        S.op("dve", lambda: nc.vector.tensor_tensor(out=out, in0=h[:], in1=gb[1][:], op=ALU.add), reads=[b_h, b_gb], writes=[b_out])

    eps_t = P.sb(es, "eps_t", [128, 1], F32)
    S.op("dve", lambda: nc.vector.memset(eps_t[:], 1e-5), writes=[b_const])

    def phase_sgu(l):
        with ExitStack() as pes:
            gt = P.sb(pes, "sg_g", [128, D], F32)
            bt = P.sb(pes, "sg_b", [128, D], F32)
            bsb = P.sb(pes, "sg_bs", [128, 4, 128], F32)
            wsr = P.sb(pes, "sg_wsr", [128, 4, 128], BF16)
            wsT = P.sb(pes, "sg_wsT", [128, 4, 128], BF16)
            b_w = S.buf("sgw")
            S.dma("sp", [(gt[:], sgu_g_d[l].partition_broadcast(128)), (bt[:], sgu_b_d[l].partition_broadcast(128)),
                         (bsb[:].rearrange("p g q -> p (g q)"), sgu_bs_d[l].partition_broadcast(128))], writes=[b_w], key="sgw")
            S.dma("pool", [(wsr[:], sgu_ws_d[l].rearrange("g p q -> p g q"))], writes=[b_w], key="sgw2")
            pst = P.ps[7].bitcast(BF16)
            for g in range(4):
                S.op("pe", lambda g=g: nc.tensor.transpose(pst[:, g * 128:(g + 1) * 128], wsr[:, g, :], ident[:]),
                     reads=[b_w, b_const], writes=[P.b_ps[7]], signal=(g == 3))
            S.op("dve", lambda: nc.vector.tensor_copy(out=wsT[:].rearrange("q g p -> q (g p)"), in_=pst[:, 0:512]), reads=[P.b_ps[7]], writes=[b_w])
            vt = [P.sb(pes, "sg_v%d" % i, [128, D], BF16) for i in range(2)]
            b_vt = S.bufs_n("sgv", 2)
            hh = [P.sb(pes, "sg_h%d" % i, [128, D], F32) for i in range(2)]
            b_hh = S.bufs_n("sgh", 2)
            vn = [P.sb(pes, "sg_vn%d" % i, [128, D], BF16) for i in range(2)]
            b_vn = S.bufs_n("sgvn", 2)
            ut = [P.sb(pes, "sg_u%d" % i, [128, 16, 512], BF16) for i in range(2)]
            b_ut = S.bufs_n("sgu", 2)
            ot = [P.sb(pes, "sg_o%d" % i, [128, 16, 512], BF16) for i in range(2)]
            b_ot = S.bufs_n("sgo", 2)
            tmp = [P.sb(pes, "sg_t%d" % i, [128, 4, 128], F32) for i in range(2)]
            b_tmp = S.bufs_n("sgt", 2)
            st6 = P.sb(pes, "sg_st6", [128, 4, 6], F32)
            mv = P.sb(pes, "sg_mv", [128, 4], F32)
            b_small = S.buf("sgsmall")
            for tb in range(NTB):
                sb_ = tb % 2
                S.dma("sp", [(ut[sb_][:, a * 4:(a + 1) * 4, :], uT[a * 512:(a + 1) * 512, tb * 512:(tb + 1) * 512].rearrange("(j p) t -> p j t", p=128))
                             for a in range(4)], writes=[b_ut[sb_]], key="sgu%d" % sb_)
                for ci in range(4):
                    tt = tb * 4 + ci
                    s2 = tt % 2
                    S.dma("sp", [(vt[s2][:], vtok[tt * 128:(tt + 1) * 128, :])], writes=[b_vt[s2]], key="sgv%d" % s2)
                    S.op("act", lambda s2=s2: nc.scalar.activation(out=hh[s2][:], in_=vt[s2][:], func=AF.Copy), reads=[b_vt[s2]], writes=[b_hh[s2]])
                    ln_rows((st6, mv, b_small), hh[s2], vn[s2][:], (gt, bt), b_hh[s2], b_vn[s2], b_w)
                    for g in range(4):
                        q = g % 4
                        for ct in range(4):
                            S.op("pe", lambda g=g, ct=ct, q=q, s2=s2: nc.tensor.matmul(P.ps[q][:, ct * 128:(ct + 1) * 128], vn[s2][:, g * 512 + ct * 128:g * 512 + (ct + 1) * 128],
                                                                                 wsT[:, g, :], start=True, stop=True),
                                 reads=[b_vn[s2], b_w], writes=[P.b_ps[q]], signal=(ct == 3))
                        j = g % 2
                        S.op("dve", lambda g=g, q=q, j=j: nc.vector.tensor_tensor(out=tmp[j][:], in0=P.ps[q].rearrange("p (c t) -> p c t", c=4),
                                                                                in1=bsb[:, g:g + 1, :].broadcast_to([128, 4, 128]), op=ALU.add),
                             reads=[P.b_ps[q], b_w], writes=[b_tmp[j]])
                        S.op("dve", lambda g=g, j=j, sb_=sb_, ci=ci: nc.vector.tensor_tensor(out=ot[sb_][:, g * 4:(g + 1) * 4, ci * 128:(ci + 1) * 128], in0=tmp[j][:],
                                                                                         in1=ut[sb_][:, g * 4:(g + 1) * 4, ci * 128:(ci + 1) * 128], op=ALU.mult),
                             reads=[b_tmp[j], b_ut[sb_]], writes=[b_ot[sb_]])
                S.dma("sp", [(sguT[a * 512:(a + 1) * 512, tb * 512:(tb + 1) * 512].rearrange("(j p) t -> p j t", p=128), ot[sb_][:, a * 4:(a + 1) * 4, :])
                             for a in range(4)], reads=[b_ot[sb_]], key="sgo%d" % sb_)
            S.barrier()

    def phase_fnet(l):
        with ExitStack() as pes:
            cc = P.sb(pes, "fn_cc", [128, 2, 4, 512], BF16)
            b_cc = S.buf("fncc")
            S.dma("sp", [(cc[:, i, :, :], dft_cc[i].rearrange("(c p) m -> p c m", p=128)) for i in range(2)], writes=[b_cc], key="fncc")
            ft = [P.sb(pes, "fn_f%d" % i, [128, 16, 512], BF16) for i in range(2)]
            b_ft = S.bufs_n("fnf", 2)
            go = [P.sb(pes, "fn_go%d" % i, [128, 2, D], BF16) for i in range(2)]
            b_go = S.bufs_n("fngo", 2)
            for tb in range(NTB):
                sb_ = tb % 2
                S.dma("sp", [(ft[sb_][:, a * 4:(a + 1) * 4, :], fT[a * 512:(a + 1) * 512, tb * 512:(tb + 1) * 512].rearrange("(j p) t -> p j t", p=128))
                             for a in range(4)], writes=[b_ft[sb_]], key="fnf%d" % sb_)
                for ci in range(4):
                    tt = tb * 4 + ci
                    s2 = tt % 2
                    for g in range(4):
                        for i in range(2):
                            q = (g * 2 + i) % 4
                            for k in range(4):
                                S.op("pe", lambda g=g, i=i, k=k, q=q, ci=ci, sb_=sb_: nc.tensor.matmul(P.ps[q], ft[sb_][:, g * 4 + k, ci * 128:(ci + 1) * 128], cc[:, i, k, :],
                                                                                              start=(k == 0), stop=(k == 3)),
                                     reads=[b_ft[sb_], b_cc], writes=[P.b_ps[q]], signal=(k == 3))
                            evac(g * 2 + i, go[s2][:, i, g * 512:(g + 1) * 512], P.ps[q], [P.b_ps[q]], [b_go[s2]])
                    S.dma("sp", [(Gcs[i][tt * 128:(tt + 1) * 128, :], go[s2][:, i, :]) for i in range(2)], reads=[b_go[s2]], key="fngo%d" % s2)
            S.barrier()
        with ExitStack() as pes:
            Gq = P.sb(pes, "fn_G", [128, 2, NTT, 512], BF16)
            b_G = S.buf("fnG")
            pn = [P.sb(pes, "fn_pn%d" % i, [128, 2, NTT, 512], BF16) for i in range(2)]
            b_pn = S.bufs_n("fnpn", 2)
            yo = [P.sb(pes, "fn_yo%d" % i, [128, 512], BF16) for i in range(4)]
            b_yo = S.bufs_n("fnyo", 4)
            it = 0
            for mq in range(4):
                S.dma("sp", [(Gq[:, i, a * 8:(a + 1) * 8, :], Gcs[i][a * 1024:(a + 1) * 1024, mq * 512:(mq + 1) * 512].rearrange("(st p) m -> p st m", p=128))
                             for i in range(2) for a in range(4)], writes=[b_G], key="fnG")
                for kb in range(NTB):
                    sl = it % 2
                    it += 1
                    S.dma("sp", [(pn[sl][:, i, a * 8:(a + 1) * 8, :], dft_s[i][a * 1024:(a + 1) * 1024, kb * 512:(kb + 1) * 512].rearrange("(st p) k -> p st k", p=128))
                                 for i in range(2) for a in range(4)], writes=[b_pn[sl]], key="fnpn%d" % sl)
                    for mt in range(4):
                        q = mt % 4
                        n = 0
                        for i in range(2):
                            for st_ in range(NTT):
                                S.op("pe", lambda i=i, st_=st_, mt=mt, q=q, sl=sl, n=n: nc.tensor.matmul(P.ps[q], Gq[:, i, st_, mt * 128:(mt + 1) * 128], pn[sl][:, i, st_, :],
                                                                                                start=(n == 0), stop=(n == 2 * NTT - 1)),
                                     reads=[b_G, b_pn[sl]], writes=[P.b_ps[q]], signal=(n == 2 * NTT - 1))
                                n += 1
                        evac(mt, yo[q][:], P.ps[q], [P.b_ps[q]], [b_yo[q]])
                        S.dma("sp", [(fnT[(mq * 4 + mt) * 128:(mq * 4 + mt + 1) * 128, kb * 512:(kb + 1) * 512], yo[q][:])], reads=[b_yo[q]], key="fnyo%d" % q)
            S.barrier()


    w_mla_o = P.dt("w_mla_o", [DEPTH, D, D], F32, EI)
    w_sgu_o = P.dt("w_sgu_o", [DEPTH, D, D], F32, EI)
    w_fnet_o = P.dt("w_fnet_o", [DEPTH, D, D], F32, EI)
    w_out = P.dt("w_out", [DEPTH, D, D], F32, EI)
    w_cq = P.dt("w_cq", [DEPTH, D, D], F32, EI)
    w_ck = P.dt("w_ck", [DEPTH, D, D], F32, EI)
    w_cv = P.dt("w_cv", [DEPTH, D, D], F32, EI)
    w_co = P.dt("w_co", [DEPTH, D, D], F32, EI)
    ln_gb = {k: P.dt(k, [DEPTH, D], F32, EI) for k in ("ln1_g", "ln1_b", "ln2_g", "ln2_b", "ln3_g", "ln3_b")}
    mem_in = P.dt("mem_in", [2 * MEM, D], F32, EI)
    yabc = [P.dt("y_%s" % c, [D, T], BF16) for c in "abc"]
    hpre = P.dt("hpre", [T, D], F32)
    x1 = P.dt("x1", [T, D], F32)
    x2 = P.dt("x2", [T, D], F32)
    x2b = P.dt("x2b", [T, D], BF16)
    qcT = P.dt("qcT", [D, T], BF16)
    kcT = P.dt("kcT", [D, 2 * MEM], BF16)
    vcm = P.dt("vcm", [2 * MEM, D], BF16)
    ocT = P.dt("ocT", [D, T], BF16)

    def load_A(A, b_A, srcs, pes):
        S.dma("sp", [(A[:, a * 4:(a + 1) * 4, :], srcs[0][a * 512:(a + 1) * 512, :].rearrange("(j p) t -> p j t", p=128)) for a in range(4)],
              writes=[b_A], key="ldA")
        if len(srcs) > 1:
            tmp = [P.sb(pes, "ldA_t%d" % i, [128, T], BF16) for i in range(2)]
            b_tmp = S.bufs_n("ldAt", 2)
            n = 0
            for j in range(16):
                for src in srcs[1:]:
                    i = n % 2
                    n += 1
                    S.dma("sp", [(tmp[i][:], src[j * 128:(j + 1) * 128, :])], writes=[b_tmp[i]], key="ldAt%d" % i)
                    S.op("dve", lambda i=i, j=j: nc.vector.tensor_tensor(out=A[:, j, :], in0=A[:, j, :], in1=tmp[i][:], op=ALU.add),
                         reads=[b_tmp[i]], writes=[b_A])

    def phase_branch_proj(l):
        for bi, (src, W) in enumerate([(oT, w_mla_o), (sguT, w_sgu_o), (fnT, w_fnet_o)]):
            with ExitStack() as pes:
                A = P.sb(pes, "bpA", [128, 16, T], BF16)
                b_A = S.buf("bpA")
                load_A(A, b_A, [src], pes)
                G = Gemm(P, S, pes, 16)
                gt = [P.sb(pes, "bp_g%d" % i, [128, T], BF16) for i in range(2)]
                b_gt = S.bufs_n("bpg", 2)
                yo = [P.sb(pes, "bp_y%d" % i, [128, T], BF16) for i in range(2)]
                b_yo = S.bufs_n("bpy", 2)
                dst = yabc[bi]

                def epi(q, m, n0, tb, width, bi=bi, dst=dst):
                    j = n0 % 2
                    if tb == 0:
                        S.dma("sp", [(gt[j][:], gT[bi * D + n0 * 128:bi * D + (n0 + 1) * 128, :])], writes=[b_gt[j]], key="bpg%d" % j)
                    S.op("dve", lambda: nc.vector.tensor_tensor(out=yo[j][:, tb * 512:(tb + 1) * 512], in0=P.ps[q], in1=gt[j][:, tb * 512:(tb + 1) * 512], op=ALU.mult),
                         reads=[P.b_ps[q], b_gt[j]], writes=[b_yo[j]])
                    if tb == NTB - 1:
                        S.dma("sp", [(dst[n0 * 128:(n0 + 1) * 128, :], yo[j][:])], reads=[b_yo[j]], key="bpy%d" % j)
                G.run(A, b_A, W[l], D, "fm", epi)
                S.barrier()

    def phase_proj_ln(l, srcs, W, xres, gname, bname, dst, dst_b=None):
        with ExitStack() as pes:
            A = P.sb(pes, "plA", [128, 16, T], BF16)
            b_A = S.buf("plA")
            load_A(A, b_A, srcs, pes)
            G = Gemm(P, S, pes, 16)
            xt = [P.sb(pes, "pl_x%d" % i, [128, 512], F32) for i in range(4)]
            b_xt = S.bufs_n("plx", 4)
            ho = [P.sb(pes, "pl_h%d" % i, [128, 512], F32) for i in range(4)]
            b_ho = S.bufs_n("plh", 4)

            def epi(q, m, n0, tt, width):
                j = tt % 4
                cs = slice(n0, n0 + 512)
                S.dma("sp", [(xt[j][:], xres[tt * 128:(tt + 1) * 128, cs])], writes=[b_xt[j]], key="plx%d" % j)
                S.op("dve", lambda: nc.vector.scalar_tensor_tensor(out=ho[j][:], in0=xt[j][:], scalar=ALPHA, in1=P.ps[q], op0=ALU.mult, op1=ALU.add),
                     reads=[b_xt[j], P.b_ps[q]], writes=[b_ho[j]])
                S.dma("sp", [(hpre[tt * 128:(tt + 1) * 128, cs], ho[j][:])], reads=[b_ho[j]], key="plh%d" % j)
            G.run(A, b_A, W[l], D, "tm", epi)
            S.barrier()
        phase_ln(l, hpre, gname, bname, dst, dst_b)

    def phase_ln(l, src, gname, bname, dst, dst_b=None):
        with ExitStack() as pes:
            gt = P.sb(pes, "ln_g", [128, D], F32)
            bt = P.sb(pes, "ln_b", [128, D], F32)
            b_gb = S.buf("lngb")
            S.dma("sp", [(gt[:], ln_gb[gname][l].partition_broadcast(128)), (bt[:], ln_gb[bname][l].partition_broadcast(128))], writes=[b_gb], key="lngb")
            hh = [P.sb(pes, "ln_h%d" % i, [128, D], F32) for i in range(3)]
            b_hh = S.bufs_n("lnh", 3)
            oo = [P.sb(pes, "ln_o%d" % i, [128, D], F32) for i in range(3)]
            b_oo = S.bufs_n("lno", 3)
            ob = [P.sb(pes, "ln_ob%d" % i, [128, D], BF16) for i in range(2)]
            b_ob = S.bufs_n("lnob", 2)
            st6 = P.sb(pes, "ln_st6", [128, 4, 6], F32)
            mv = P.sb(pes, "ln_mv", [128, 4], F32)
            b_small = S.buf("lnsmall")
            for tt in range(NTT):
                j = tt % 3
                rows = slice(tt * 128, (tt + 1) * 128)
                S.dma("sp", [(hh[j][:], src[rows, :])], writes=[b_hh[j]], key="lnh%d" % j)
                ln_rows((st6, mv, b_small), hh[j], oo[j][:], (gt, bt), b_hh[j], b_oo[j], b_gb)
                S.dma("sp", [(dst[rows, :], oo[j][:])], reads=[b_oo[j]], key="lno%d" % j)
                if dst_b is not None:
                    jb = tt % 2
                    S.op("act", lambda j=j, jb=jb: nc.scalar.activation(out=ob[jb][:], in_=oo[j][:], func=AF.Copy), reads=[b_oo[j]], writes=[b_ob[jb]])
                    S.dma("sp", [(dst_b[rows, :], ob[jb][:])], reads=[b_ob[jb]], key="lnob%d" % jb)
            S.barrier()

    XSCALE = 512 ** -0.5

    def phase_cross_qkv(l):
        with ExitStack() as pes:
            xT = P.sb(pes, "cxT", [128, 16, T], BF16)
            b_xT = S.buf("cxT")
            G = Gemm(P, S, pes, 16)
            build_xT(xT, b_xT, G.wp, G.b_wp, x1, T)
            yo = [P.sb(pes, "cq_y%d" % i, [128, T], BF16) for i in range(2)]
            b_yo = S.bufs_n("cqy", 2)

            def epi(q, m, n0, tb, width):
                j = n0 % 2
                evac(tb, yo[j][:, tb * 512:(tb + 1) * 512], P.ps[q], [P.b_ps[q]], [b_yo[j]])
                if tb == NTB - 1:
                    S.dma("sp", [(qcT[n0 * 128:(n0 + 1) * 128, :], yo[j][:])], reads=[b_yo[j]], key="cqy%d" % j)
            G.run(xT, b_xT, w_cq[l], D, "fm", epi)
            S.barrier()
        with ExitStack() as pes:
            mT = P.sb(pes, "cmT", [128, 16, 2 * MEM], BF16)
            b_mT = S.buf("cmT")
            G = Gemm(P, S, pes, 16)
            build_xT(mT, b_mT, G.wp, G.b_wp, mem_in, 2 * MEM)
            ko = [P.sb(pes, "ck_y%d" % i, [128, 512], BF16) for i in range(4)]
            b_ko = S.bufs_n("cky", 4)

            def epi_k(q, m, n0, tb, width):
                j = n0 % 4
                evac(n0, ko[j][:], P.ps[q], [P.b_ps[q]], [b_ko[j]])
                S.dma("sp", [(kcT[n0 * 128:(n0 + 1) * 128, :], ko[j][:])], reads=[b_ko[j]], key="cky%d" % j)
            G.run(mT, b_mT, w_ck[l], D, "fm", epi_k, Tc=2 * MEM)

            def epi_v(q, m, n0, tt, width):
                j = tt % 4
                evac(tt, ko[j][:], P.ps[q], [P.b_ps[q]], [b_ko[j]])
                S.dma("sp", [(vcm[tt * 128:(tt + 1) * 128, n0:n0 + 512], ko[j][:])], reads=[b_ko[j]], key="cky%d" % j)
            G.run(mT, b_mT, w_cv[l], D, "tm", epi_v, Tc=2 * MEM)
            S.barrier()

    def phase_cross_attn(l):
        with ExitStack() as pes:
            kc = P.sb(pes, "xa_k", [128, 16, 2 * MEM], BF16)
            vc = P.sb(pes, "xa_v", [128, 4, D], BF16)
            b_kv = S.buf("xakv")
            S.dma("sp", [(kc[:, a * 4:(a + 1) * 4, :], kcT[a * 512:(a + 1) * 512, :].rearrange("(j p) m -> p j m", p=128)) for a in range(4)]
                  + [(vc[:], vcm.rearrange("(a p) d -> p a d", p=128))], writes=[b_kv], key="xakv")
            qh = [P.sb(pes, "xa_q%d" % i, [128, 4, T], BF16) for i in range(2)]
            b_qh = S.bufs_n("xaq", 2)
            sq = P.sb(pes, "xa_sq", [128, 4, T], BF16)
            b_sq = S.buf("xasq")
            mx = P.sb(pes, "xa_mx", [128, 32], F32)
            b_mx = S.buf("xamx")
            Pt = [P.sb(pes, "xa_P%d" % i, [128, 2, 512], BF16) for i in range(2)]
            b_pt = S.bufs_n("xaP", 2)
            rinv = [P.sb(pes, "xa_r%d" % i, [128, 512], F32) for i in range(2)]
            b_rinv = S.bufs_n("xar", 2)
            osb = [P.sb(pes, "xa_o%d" % i, [128, 4, T], BF16) for i in range(2)]
            b_osb = S.bufs_n("xao", 2)
            b_pp = S.bufs_n("xapp", 2)

            def load_q(h):
                s = h % 2
                S.dma("sp", [(qh[s][:], qcT[h * 512:(h + 1) * 512, :].rearrange("(j p) t -> p j t", p=128))], writes=[b_qh[s]], key="xaq%d" % s)
            load_q(0)
            for h in range(4):
                s = h % 2
                if h + 1 < 4:
                    load_q(h + 1)
                S.op("act", lambda: nc.scalar.activation(out=sq[:], in_=qh[s][:], func=AF.Square), reads=[b_qh[s]], writes=[b_sq])
                for c in range(NTB):
                    q = 6 + c % 2
                    for k in range(4):
                        S.op("pe", lambda c=c, k=k, q=q: nc.tensor.matmul(P.ps[q], ones[:], sq[:, k, c * 512:(c + 1) * 512], start=(k == 0), stop=(k == 3)),
                             reads=[b_sq, b_const], writes=[P.b_ps[q]], signal=(k == 3))
                    S.op("dve", lambda c=c, q=q: nc.vector.reduce_max(out=mx[:, c:c + 1], in_=P.ps[q], axis=AX.X), reads=[P.b_ps[q]], writes=[b_mx])
                S.op("act", lambda: nc.scalar.activation(out=sq[:, :, 0:512], in_=kc[:, h * 4:(h + 1) * 4, :], func=AF.Square), reads=[b_kv, b_sq], writes=[b_sq])
                for k in range(4):
                    S.op("pe", lambda k=k: nc.tensor.matmul(P.ps[6], ones[:], sq[:, k, 0:512], start=(k == 0), stop=(k == 3)),
                         reads=[b_sq, b_const], writes=[P.b_ps[6]], signal=(k == 3))
                S.op("dve", lambda: nc.vector.reduce_max(out=mx[:, 17:18], in_=P.ps[6], axis=AX.X), reads=[P.b_ps[6]], writes=[b_mx])
                S.op("dve", lambda: nc.vector.reduce_max(out=mx[:, 16:17], in_=mx[:, 0:8], axis=AX.X), reads=[b_mx], writes=[b_mx])
                S.op("dve", lambda: nc.vector.tensor_tensor(out=mx[:, 18:19], in0=mx[:, 16:17], in1=mx[:, 17:18], op=ALU.mult), reads=[b_mx], writes=[b_mx])
                S.op("act", lambda: nc.scalar.activation(out=mx[:, 19:20], in_=mx[:, 18:19], func=AF.Sqrt, scale=XSCALE * XSCALE), reads=[b_mx], writes=[b_mx])
                S.op("dve", lambda: nc.vector.tensor_scalar(out=mx[:, 20:21], in0=mx[:, 19:20], scalar1=-1.0, scalar2=None, op0=ALU.mult), reads=[b_mx], writes=[b_mx])
                for tb in range(NTB):
                    seq = tb // 4
                    j = tb % 2
                    ts_ = slice(tb * 512, (tb + 1) * 512)
                    for mt in range(2):
                        for c in range(4):
                            S.op("pe", lambda mt=mt, c=c: nc.tensor.matmul(P.pst[:, j * 2 + mt, :], kc[:, h * 4 + c, seq * MEM + mt * 128:seq * MEM + (mt + 1) * 128],
                                                                           qh[s][:, c, ts_], start=(c == 0), stop=(c == 3)),
                                 reads=[b_kv, b_qh[s]], writes=[b_pp[j]], signal=(c == 3 and mt == 1))
                    S.op("act", lambda: nc.scalar.activation(out=Pt[j][:], in_=P.pst[:, j * 2:j * 2 + 2, :], func=AF.Exp, scale=XSCALE, bias=mx[:, 20:21]),
                         reads=[b_pp[j], b_mx], writes=[b_pt[j]])
                    for mt in range(2):
                        S.op("pe", lambda mt=mt: nc.tensor.matmul(P.ps[4 + j], ones[:], Pt[j][:, mt, :], start=(mt == 0), stop=(mt == 1)),
                             reads=[b_pt[j], b_const], writes=[P.b_ps[4 + j]], signal=(mt == 1))
                    S.op("dve", lambda: nc.vector.reciprocal(out=rinv[j][:], in_=P.ps[4 + j]), reads=[P.b_ps[4 + j]], writes=[b_rinv[j]])
                    for dt_ in range(4):
                        q = 6 + dt_ % 2
                        for mt in range(2):
                            S.op("pe", lambda mt=mt, dt_=dt_, q=q: nc.tensor.matmul(P.ps[q], vc[:, seq * 2 + mt, h * 512 + dt_ * 128:h * 512 + (dt_ + 1) * 128],
                                                                                Pt[j][:, mt, :], start=(mt == 0), stop=(mt == 1)),
                                 reads=[b_kv, b_pt[j]], writes=[P.b_ps[q]], signal=(mt == 1))
                        S.op("dve", lambda dt_=dt_, q=q: nc.vector.tensor_tensor(out=osb[s][:, dt_, ts_], in0=P.ps[q], in1=rinv[j][:], op=ALU.mult),
                             reads=[P.b_ps[q], b_rinv[j]], writes=[b_osb[s]])
                S.dma("sp", [(ocT[h * 512:(h + 1) * 512, :].rearrange("(j p) t -> p j t", p=128), osb[s][:])], reads=[b_osb[s]], key="xao%d" % s)
            S.barrier()


    w_router = P.dt("w_router", [DEPTH, D, NEXP], F32, EI)
    b_router = P.dt("b_router", [DEPTH, NEXP], F32, EI)
    w_gu = P.dt("w_gu", [DEPTH, NEXP, D, 2 * D], F32, EI)
    b_gu = P.dt("b_gu", [DEPTH, NEXP, 128, 32], F32, EI)
    w_down = P.dt("w_down", [DEPTH, NEXP, D, D], F32, EI)
    b_down = P.dt("b_down", [DEPTH, NEXP, D], F32, EI)
    utri_d = P.dt("utri", [128, 128], BF16, EI)
    ecap_d = P.dt("ecap", [128, NEXP], F32, EI)
    tokid_d = P.dt("tokid", [128, NTT], I32, EI)
    slot_tok = P.dt("slot_tok", [NEXP * CAP, 1], I32)
    tok_slot = P.dt("tok_slot", [T, 4], I32)
    tok_prob = P.dt("tok_prob", [T, 4], F32)
    yslot = P.dt("yslot", [NEXP * CAP, D], BF16)
    x_l1 = P.dt("x_l1", [T, D], F32)
    y_out = P.dt("y_out", [T, D], F32, "ExternalOutput")

    def phase_router(l):
        with ExitStack() as pes:
            xT = P.sb(pes, "rxT", [128, 16, T], BF16)
            b_xT = S.buf("rxT")
            G = Gemm(P, S, pes, 16)
            build_xT(xT, b_xT, G.wp, G.b_wp, x2, T)
            wr = P.sb(pes, "r_wr", [128, 16, NEXP], BF16)
            br = P.sb(pes, "r_br", [128, NEXP], F32)
            utri = P.sb(pes, "r_utri", [128, 128], BF16)
            ecap = P.sb(pes, "r_ecap", [128, NEXP], F32)
            tokid = P.sb(pes, "r_tokid", [128, NTT], I32)
            sent = P.sb(pes, "r_sent", [128, NEXP * CAP // 128], I32)
            b_rc = S.buf("rconst")
            S.dma("pool", [(wr[:], w_router[l].rearrange("(c p) e -> p c e", p=128))], writes=[b_rc], key="rc1")
            S.dma("sp", [(br[:], b_router[l].partition_broadcast(128)), (utri[:], utri_d[:, :]), (ecap[:], ecap_d[:, :]), (tokid[:], tokid_d[:, :])],
                  writes=[b_rc], key="rc2")
            S.op("dve", lambda: nc.vector.memset(sent[:], T), writes=[b_rc])
            b_slottok = S.buf("slottok")
            S.dma("sp", [(slot_tok.rearrange("(p a) o -> p (a o)", p=128), sent[:])], reads=[b_rc], writes=[b_slottok], key="rc3")
            carry = P.sb(pes, "r_carry", [128, NEXP], F32)
            b_carry = S.buf("rcarry")
            S.op("dve", lambda: nc.vector.memset(carry[:], 0.0), writes=[b_carry])
            NB = 2
            lg = [P.sb(pes, "r_lg%d" % i, [128, NEXP], F32) for i in range(NB)]
            t8 = [P.sb(pes, "r_t8%d" % i, [128, 16], F32) for i in range(NB)]
            mk = [P.sb(pes, "r_mk%d" % i, [128, NEXP], BF16) for i in range(NB)]
            se = [P.sb(pes, "r_se%d" % i, [128, NEXP], F32) for i in range(NB)]
            junk = [P.sb(pes, "r_junk%d" % i, [128, NEXP], F32) for i in range(NB)]
            pk = [P.sb(pes, "r_pk%d" % i, [128, 8], F32) for i in range(NB)]
            slf = [P.sb(pes, "r_slf%d" % i, [128, 4], F32) for i in range(NB)]
            sli = [P.sb(pes, "r_sli%d" % i, [128, 4], I32) for i in range(NB)]
            b_t = S.bufs_n("rt", NB)
            for tt in range(NTT):
                i = tt % NB
                q = tt % 2
                bt_ = b_t[i]
                for k in range(16):
                    S.op("pe", lambda k=k, q=q: nc.tensor.matmul(P.ps[q][:, 0:NEXP], xT[:, k, tt * 128:(tt + 1) * 128], wr[:, k, :], start=(k == 0), stop=(k == 15)),
                         reads=[b_xT, b_rc], writes=[P.b_ps[q]], signal=(k == 15))
                S.op("dve", lambda: nc.vector.tensor_tensor(out=lg[i][:], in0=P.ps[q][:, 0:NEXP], in1=br[:], op=ALU.add), reads=[P.b_ps[q], b_rc], writes=[bt_])
                S.op("dve", lambda: nc.vector.max(out=t8[i][:, 0:8], in_=lg[i][:]), reads=[bt_], writes=[bt_])
                S.op("dve", lambda: nc.vector.tensor_scalar(out=mk[i][:], in0=lg[i][:], scalar1=t8[i][:, 3:4], scalar2=None, op0=ALU.is_ge), reads=[bt_], writes=[bt_])
                S.op("dve", lambda: nc.vector.tensor_scalar(out=t8[i][:, 8:9], in0=t8[i][:, 0:1], scalar1=-1.0, scalar2=None, op0=ALU.mult), reads=[bt_], writes=[bt_])
                S.op("act", lambda: nc.scalar.activation(out=pk[i][:, 0:4], in_=t8[i][:, 0:4], func=AF.Exp, bias=t8[i][:, 8:9], accum_out=pk[i][:, 4:5]),
                     reads=[bt_], writes=[bt_])
                S.op("dve", lambda: nc.vector.reciprocal(out=pk[i][:, 5:6], in_=pk[i][:, 4:5]), reads=[bt_], writes=[bt_])
                S.op("dve", lambda: nc.vector.tensor_scalar(out=pk[i][:, 0:4], in0=pk[i][:, 0:4], scalar1=pk[i][:, 5:6], scalar2=None, op0=ALU.mult), reads=[bt_], writes=[bt_])
                qr, qt = 2 + tt % 2, 4 + tt % 2
                S.op("pe", lambda: nc.tensor.matmul(P.ps[qr][:, 0:NEXP], utri[:], mk[i][:], start=True, stop=True), reads=[bt_, b_rc], writes=[P.b_ps[qr]])
                S.op("pe", lambda: nc.tensor.matmul(P.ps[qt][:, 0:NEXP], ones[:], mk[i][:], start=True, stop=True), reads=[bt_, b_const], writes=[P.b_ps[qt]])
                S.op("dve", lambda: nc.vector.tensor_tensor(out=se[i][:], in0=P.ps[qr][:, 0:NEXP], in1=carry[:], op=ALU.add), reads=[P.b_ps[qr], b_carry], writes=[bt_])
                S.op("dve", lambda: nc.vector.tensor_tensor(out=carry[:], in0=P.ps[qt][:, 0:NEXP], in1=carry[:], op=ALU.add), reads=[P.b_ps[qt]], writes=[b_carry])
                S.op("dve", lambda: nc.vector.scalar_tensor_tensor(out=se[i][:], in0=se[i][:], scalar=float(CAP - 1), in1=ecap[:], op0=ALU.min, op1=ALU.add),
                     reads=[b_rc], writes=[bt_])
                for k in range(4):
                    S.op("dve", lambda k=k: nc.vector.scalar_tensor_tensor(out=junk[i][:], in0=lg[i][:], scalar=t8[i][:, k:k + 1], in1=se[i][:], op0=ALU.is_equal, op1=ALU.mult,
                                                                         accum_out=slf[i][:, k:k + 1]), reads=[bt_], writes=[bt_])
                S.op("dve", lambda: nc.vector.tensor_copy(out=sli[i][:], in_=slf[i][:]), reads=[bt_], writes=[bt_])
                rows = slice(tt * 128, (tt + 1) * 128)
                S.dma("sp", [(tok_slot[rows, :], sli[i][:]), (tok_prob[rows, :], pk[i][:, 0:4])], reads=[bt_], key="rst%d" % i)
                for k in range(4):
                    S.dma_raw("pool", lambda k=k: nc.gpsimd.indirect_dma_start(out=slot_tok, out_offset=bass.IndirectOffsetOnAxis(ap=sli[i][:, k:k + 1], axis=0),
                                                                              in_=tokid[:, tt:tt + 1], in_offset=None),
                              reads=[bt_, b_rc], writes=[b_slottok], key="rsc")
            S.barrier()

    bc_reg = nc.gpsimd.alloc_register("bcreg")
    nc.gpsimd.reg_mov(bc_reg, T - 1)

    def phase_experts(l):
        with ExitStack() as pes:
            NH = CAP // 2
            idx = [P.sb(pes, "e_idx%d" % i, [128, CAP // 128], I32) for i in range(2)]
            b_idx = S.bufs_n("eidx", 2)
            xg = [P.sb(pes, "e_xg%d" % i, [128, D], BF16) for i in range(2)]
            b_xg = S.bufs_n("exg", 2)
            for i in range(2):
                S.op("dve", lambda i=i: nc.vector.memset(xg[i][:], 0.0), writes=[b_xg[i]])
            xgT = P.sb(pes, "e_xgT", [128, 16, CAP], BF16)
            b_xgT = S.buf("exgT")
            actT = P.sb(pes, "e_actT", [128, 16, CAP], BF16)
            b_actT = S.buf("eactT")
            wg = [P.sb(pes, "e_wg%d" % i, [128, 2, 16, 512], BF16) for i in range(2)]
            b_wg = S.bufs_n("ewg", 2)
            wd = [P.sb(pes, "e_wd%d" % i, [128, 16, 512], BF16) for i in range(2)]
            b_wd = S.bufs_n("ewd", 2)
            bgu = [P.sb(pes, "e_bgu%d" % i, [128, 32], F32) for i in range(2)]
            bdn = [P.sb(pes, "e_bdn%d" % i, [1, D], BF16) for i in range(2)]
            b_bias = S.bufs_n("ebias", 2)
            gc = [P.sb(pes, "e_gc%d" % i, [128, NH], F32) for i in range(2)]
            sg = [P.sb(pes, "e_sg%d" % i, [128, NH], F32) for i in range(2)]
            uc = [P.sb(pes, "e_uc%d" % i, [128, NH], F32) for i in range(2)]
            b_ep = S.bufs_n("eep", 2)
            yo = [P.sb(pes, "e_yo%d" % i, [128, 512], BF16) for i in range(4)]
            b_yo = S.bufs_n("eyo", 4)
            wcount = [0, 0]
            for e in range(NEXP):
                eb = e % 2
                S.dma("sp", [(bgu[eb][:], b_gu[l, e]), (idx[eb][:], slot_tok[e * CAP:(e + 1) * CAP, :].rearrange("(p a) o -> p (a o)", p=128))],
                      writes=[b_bias[eb], b_idx[eb]], key="eb%d" % eb)
                S.dma("pool", [(bdn[eb][:], b_down[l, e:e + 1, :])], writes=[b_bias[eb]], key="ebd%d" % eb)
                for st_ in range(CAP // 128):
                    gi = st_ % 2
                    S.dma_raw("pool", lambda st_=st_, gi=gi: nc.gpsimd.indirect_dma_start(
                        out=xg[gi][:], out_offset=None, in_=x2b, in_offset=bass.IndirectOffsetOnAxis(ap=idx[eb][:, st_:st_ + 1], axis=0),
                        bounds_check=bc_reg, oob_is_err=False), reads=[b_idx[eb]], writes=[b_xg[gi]], key="eg%d" % gi)
                    for half in range(2):
                        q = 6 + half
                        pst = P.ps[q].bitcast(BF16)
                        for j in range(8):
                            c = half * 8 + j
                            S.op("pe", lambda c=c, j=j, pst=pst, gi=gi: nc.tensor.transpose(pst[:, j * 128:(j + 1) * 128], xg[gi][:, c * 128:(c + 1) * 128], ident[:]),
                                 reads=[b_xg[gi], b_const], writes=[P.b_ps[q]], signal=(j == 7))
                        evac(half, xgT[:, half * 8:(half + 1) * 8, st_ * 128:(st_ + 1) * 128], pst.rearrange("p (j t) -> p j t", j=8), [P.b_ps[q]], [b_xgT])
                for jp in range(4):
                    sl = wcount[0] % 2
                    wcount[0] += 1
                    S.dma("pool", [(wg[sl][:, 0, :, :], w_gu[l, e][:, jp * 512:(jp + 1) * 512].rearrange("(c p) n -> p c n", p=128)),
                                   (wg[sl][:, 1, :, :], w_gu[l, e][:, D + jp * 512:D + (jp + 1) * 512].rearrange("(c p) n -> p c n", p=128))],
                          writes=[b_wg[sl]], key="ewg%d" % sl)
                    for sub in range(4):
                        j = jp * 4 + sub
                        for hf in range(2):
                            pi = (sub * 2 + hf) % 2
                            qg, qu = pi * 2, pi * 2 + 1
                            for gu, qq in ((0, qg), (1, qu)):
                                for k in range(16):
                                    S.op("pe", lambda gu=gu, qq=qq, k=k, sub=sub, hf=hf, sl=sl: nc.tensor.matmul(
                                        P.ps[qq][:, 0:NH], wg[sl][:, gu, k, sub * 128:(sub + 1) * 128], xgT[:, k, hf * NH:(hf + 1) * NH], start=(k == 0), stop=(k == 15)),
                                        reads=[b_wg[sl], b_xgT], writes=[P.b_ps[qq]], signal=(k == 15))
                            S.op("dve", lambda pi=pi, qg=qg, j=j: nc.vector.tensor_scalar(out=gc[pi][:], in0=P.ps[qg][:, 0:NH], scalar1=bgu[eb][:, j:j + 1], scalar2=7.0, op0=ALU.add, op1=ALU.min),
                                 reads=[P.b_ps[qg], b_bias[eb]], writes=[b_ep[pi]])
                            S.op("act", lambda pi=pi: nc.scalar.activation(out=sg[pi][:], in_=gc[pi][:], func=AF.Sigmoid, scale=1.702), reads=[b_ep[pi]], writes=[b_ep[pi]])
                            S.op("dve", lambda pi=pi, qu=qu, j=j: nc.vector.tensor_scalar(out=uc[pi][:], in0=P.ps[qu][:, 0:NH], scalar1=bgu[eb][:, 16 + j:17 + j], scalar2=7.0, op0=ALU.add, op1=ALU.min),
                                 reads=[P.b_ps[qu], b_bias[eb]], writes=[b_ep[pi]])
                            S.op("dve", lambda pi=pi: nc.vector.tensor_scalar(out=uc[pi][:], in0=uc[pi][:], scalar1=-7.0, scalar2=1.0, op0=ALU.max, op1=ALU.add), reads=[b_ep[pi]], writes=[b_ep[pi]])
                            S.op("dve", lambda pi=pi: nc.vector.tensor_tensor(out=gc[pi][:], in0=gc[pi][:], in1=sg[pi][:], op=ALU.mult), reads=[b_ep[pi]], writes=[b_ep[pi]])
                            S.op("dve", lambda pi=pi, j=j, hf=hf: nc.vector.tensor_tensor(out=actT[:, j, hf * NH:(hf + 1) * NH], in0=gc[pi][:], in1=uc[pi][:], op=ALU.mult),
                                 reads=[b_ep[pi]], writes=[b_actT])
                for dp in range(4):
                    sl = wcount[1] % 2
                    wcount[1] += 1
                    S.dma("pool", [(wd[sl][:], w_down[l, e][:, dp * 512:(dp + 1) * 512].rearrange("(c p) n -> p c n", p=128))], writes=[b_wd[sl]], key="ewd%d" % sl)
                    for st_ in range(CAP // 128):
                        q = 4 + st_ % 2
                        for k in range(16):
                            S.op("pe", lambda k=k, q=q, st_=st_, sl=sl: nc.tensor.matmul(P.ps[q], actT[:, k, st_ * 128:(st_ + 1) * 128], wd[sl][:, k, :], start=(k == 0), stop=False),
                                 reads=[b_actT, b_wd[sl]], writes=[P.b_ps[q]], signal=False)
                        S.op("pe", lambda q=q, dp=dp: nc.tensor.matmul(P.ps[q], ones[0:1, :], bdn[eb][0:1, dp * 512:(dp + 1) * 512], start=False, stop=True),
                             reads=[b_bias[eb], b_const], writes=[P.b_ps[q]])
                        yi = (dp * 6 + st_) % 4
                        evac(st_, yo[yi][:], P.ps[q], [P.b_ps[q]], [b_yo[yi]])
                        S.dma("sp", [(yslot[e * CAP:(e + 1) * CAP, dp * 512:(dp + 1) * 512].rearrange("(p a) d -> a p d", p=128)[st_], yo[yi][:])], reads=[b_yo[yi]], key="eyo%d" % yi)
            S.barrier()

    def phase_combine(l, dst):
        with ExitStack() as pes:
            gt = P.sb(pes, "c_g", [128, D], F32)
            bt = P.sb(pes, "c_b", [128, D], F32)
            b_gb = S.buf("cgb")
            S.dma("sp", [(gt[:], ln_gb["ln3_g"][l].partition_broadcast(128)), (bt[:], ln_gb["ln3_b"][l].partition_broadcast(128))], writes=[b_gb], key="cgb")
            NB = 2
            sl = [P.sb(pes, "c_sl%d" % i, [128, 4], I32) for i in range(NB)]
            pk = [P.sb(pes, "c_pk%d" % i, [128, 4], F32) for i in range(NB)]
            b_sp = S.bufs_n("csp", NB)
            yr = [P.sb(pes, "c_yr%d" % i, [128, D], BF16) for i in range(4)]
            b_yr = S.bufs_n("cyr", 4)
            hh = [P.sb(pes, "c_h%d" % i, [128, D], F32) for i in range(NB)]
            b_hh = S.bufs_n("chh", NB)
            oo = [P.sb(pes, "c_o%d" % i, [128, D], F32) for i in range(NB)]
            b_oo = S.bufs_n("coo", NB)
            st6 = P.sb(pes, "c_st6", [128, 4, 6], F32)
            mv = P.sb(pes, "c_mv", [128, 4], F32)
            b_small = S.buf("csmall")
            for tt in range(NTT):
                i = tt % NB
                rows = slice(tt * 128, (tt + 1) * 128)
                S.dma("sp", [(sl[i][:], tok_slot[rows, :]), (pk[i][:], tok_prob[rows, :])], writes=[b_sp[i]], key="csp%d" % i)
                S.dma("sp", [(oo[i][:], x2[rows, :])], writes=[b_oo[i]], key="cx%d" % i)
                S.op("act", lambda: nc.scalar.activation(out=hh[i][:], in_=oo[i][:], func=AF.Copy, scale=ALPHA), reads=[b_oo[i]], writes=[b_hh[i]])
                for k in range(4):
                    yi = (tt * 4 + k) % 4
                    S.dma_raw("pool", lambda k=k, yi=yi: nc.gpsimd.indirect_dma_start(
                        out=yr[yi][:], out_offset=None, in_=yslot, in_offset=bass.IndirectOffsetOnAxis(ap=sl[i][:, k:k + 1], axis=0)),
                        reads=[b_sp[i]], writes=[b_yr[yi]], key="cg%d" % yi)
                    S.op("dve", lambda k=k, yi=yi: nc.vector.scalar_tensor_tensor(out=hh[i][:], in0=yr[yi][:], scalar=pk[i][:, k:k + 1], in1=hh[i][:], op0=ALU.mult, op1=ALU.add),
                         reads=[b_yr[yi], b_sp[i]], writes=[b_hh[i]])
                ln_rows((st6, mv, b_small), hh[i], oo[i][:], (gt, bt), b_hh[i], b_oo[i], b_gb)
                S.dma("sp", [(dst[rows, :], oo[i][:])], reads=[b_oo[i]], key="co%d" % i)
            S.barrier()

    for l in layers:
        x_src = x_in if l == 0 else x_l1
        if want("zgemm"):
            phase_zgemm(l, x_src)
        if want("mla_prep"):
            phase_mla_prep(l)
        if want("attn"):
            phase_attn(l)
        if want("sgu"):
            phase_sgu(l)
        if want("fnet"):
            phase_fnet(l)
        if want("proj1"):
            phase_branch_proj(l)
            phase_proj_ln(l, yabc, w_out, x_src, "ln1_g", "ln1_b", x1)
        if want("cross"):
            phase_cross_qkv(l)
            phase_cross_attn(l)
            phase_proj_ln(l, [ocT], w_co, x1, "ln2_g", "ln2_b", x2, x2b)
        if want("moe"):
            phase_router(l)
            phase_experts(l)
            phase_combine(l, y_out if l == DEPTH - 1 else x_l1)

    S.finish()
    P.ninstr = S.ninstr
    return P


_PROG = {}


def _bf16():
    import ml_dtypes
    return ml_dtypes.bfloat16


def _seq_tables(segs):
    BF = _bf16()
    inv = 1.0 / (10000.0 ** (np.arange(0, 64, 2, dtype=np.float32) / 64))
    pos = np.concatenate([np.arange(L, dtype=np.float32) for L in segs])
    ang = pos[:, None] * inv[None, :]
    cos, sin = np.cos(ang).T.astype(np.float32), np.sin(ang).T.astype(np.float32)
    rope_cs = np.stack([np.concatenate([cos, cos], 0), np.concatenate([-sin, sin], 0)]).astype(np.float32)
    seg_id = np.concatenate([np.full(L, i) for i, L in enumerate(segs)])
    mask = np.zeros((128, NTT * NTB), np.float32)
    for kt in range(NTT):
        for qc in range(NTB):
            if seg_id[kt * 128] != seg_id[qc * 512]:
                mask[:, kt * NTB + qc] = -30000.0
    dft = np.zeros((2, T, T), np.float32)
    o = 0
    for L in segs:
        a = (np.outer(np.arange(L), np.arange(L)) % L).astype(np.float64) * (2 * np.pi / L)
        nrm = 1.0 / np.sqrt(L * 512)
        dft[0, o:o + L, o:o + L] = np.cos(a) * nrm
        dft[1, o:o + L, o:o + L] = -np.sin(a) * nrm
        o += L
    return rope_cs, mask, dft.astype(BF)


def _const_inputs():
    BF = _bf16()
    ac = (np.outer(np.arange(512), np.arange(512)) % 512).astype(np.float64) * (2 * np.pi / 512)
    return {
        "ident": np.eye(128, dtype=np.float32).astype(BF),
        "dft_cc": np.stack([np.cos(ac), np.sin(ac)]).astype(np.float32).astype(BF),
        "utri": np.triu(np.ones((128, 128), np.float32), 1).astype(BF),
        "ecap": np.tile((np.arange(NEXP) * CAP).astype(np.float32)[None, :], (128, 1)),
        "tokid": (np.arange(NTT)[None, :] * 128 + np.arange(128)[:, None]).astype(np.int32),
    }


def _pc(v, c):
    return np.ascontiguousarray(v.reshape(v.shape[0], c, 128).transpose(0, 2, 1))


def make_in_maps(inputs):
    f = {k: np.asarray(v) for k, v in inputs.items()}
    shared = {k: np.ascontiguousarray(f[k], dtype=np.float32) for k in (
        "w_in", "w_uq", "w_ukv", "w_mla_o", "sgu_ln_g", "sgu_ln_b", "sgu_ws", "w_sgu_o", "w_fnet_o", "w_out",
        "ln1_g", "ln1_b", "w_cq", "w_ck", "w_cv", "w_co", "ln2_g", "ln2_b", "w_router", "b_router", "w_gu",
        "w_down", "b_down", "ln3_g", "ln3_b")}
    shared["b_gate"] = np.ascontiguousarray(f["b_gate"], dtype=np.float32).reshape(DEPTH, 3 * D, 1)
    shared["mla_q_norm"] = _pc(f["mla_q_norm"].astype(np.float32), 4)
    shared["mla_kv_norm"] = _pc(f["mla_kv_norm"].astype(np.float32), 4)
    shared["sgu_bs"] = np.ascontiguousarray(f["sgu_bs"], dtype=np.float32).reshape(DEPTH, 512)
    shared["b_gu"] = np.ascontiguousarray(f["b_gu"].astype(np.float32).reshape(DEPTH, NEXP, 32, 128).transpose(0, 1, 3, 2))
    shared.update(_const_inputs())
    tab_pair = _seq_tables([2048, 2048])
    tab_samp = _seq_tables([4096])
    xp, xs, mp, ms = f["x_prompt"], f["x_sample"], f["mem_prompt"], f["mem_sample"]
    groups = []
    for c in range(4):
        groups.append(("pair", np.concatenate([xp[2 * c], xp[2 * c + 1]], 0), np.concatenate([mp[2 * c], mp[2 * c + 1]], 0)))
    for c in range(2):
        groups.append(("samp", xs[c], np.concatenate([ms[c], ms[c]], 0)))
    groups.append(groups[0])
    groups.append(groups[4])
    in_maps = []
    for kind, x, mem in groups:
        rope_cs, mask, dft = tab_pair if kind == "pair" else tab_samp
        m = dict(shared)
        m.update({"x_in": np.ascontiguousarray(x, dtype=np.float32), "mem_in": np.ascontiguousarray(mem, dtype=np.float32),
                  "rope_cs": rope_cs, "attn_mask": mask, "dft_s": dft})
        in_maps.append(m)
    return in_maps


def kernel(**inputs):
    if "P" not in _PROG:
        _PROG["P"] = build_program()
    P = _PROG["P"]
    in_maps = make_in_maps(inputs)
    res = run_bass_kernel_spmd(P.nc, in_maps, core_ids=list(range(8)))
    outs = [np.asarray(r["y_out"], dtype=np.float32) for r in res.results]
    y_prompt = np.stack([outs[c // 2][(c % 2) * 2048:(c % 2 + 1) * 2048] for c in range(8)], 0)
    y_sample = np.stack([outs[4], outs[5]], 0)
    return (y_prompt, y_sample)
```

```python
import numpy as np
from contextlib import ExitStack
import concourse.bass as bass
import concourse.mybir as mybir
from concourse.bass_utils import run_bass_kernel_spmd

F32 = mybir.dt.float32
BF16 = mybir.dt.bfloat16
I32 = mybir.dt.int32
AF = mybir.ActivationFunctionType
ALU = mybir.AluOpType
AX = mybir.AxisListType

SEM_BIAS = 20000
USE_LOOPS = False


class LE:
    __slots__ = ("c", "t")

    def __init__(self, c=0, t=None):
        self.c = c
        self.t = t or {}

    def add(self, k):
        return LE(self.c + k, self.t)

    def addvar(self, v, coef):
        t = dict(self.t)
        t[v] = t.get(v, 0) + coef
        if t[v] == 0:
            del t[v]
        return LE(self.c, t)

    def subst(self, v, val):
        if v not in self.t:
            return self
        t = dict(self.t)
        coef = t.pop(v)
        return LE(self.c + coef * val, t)

    def same(self, o):
        return self.t == o.t


class Buf:
    def __init__(self, S, name):
        self.name = name
        self.ws = []
        self.rs = []
        S.bufs.append(self)


def _prune(toks):
    out = []
    for t in toks:
        k, le, ser = t
        rep = False
        for j, (k2, le2, ser2) in enumerate(out):
            if k2 == k and le2.same(le):
                if le.c > le2.c:
                    out[j] = t
                rep = True
                break
        if not rep:
            out.append(t)
    return out


class Sched:
    NDMASEM = 40

    def __init__(self, nc, es):
        self.nc = nc
        self.es = es
        self.eng = {"pe": nc.tensor, "act": nc.scalar, "dve": nc.vector, "pool": nc.gpsimd, "sp": nc.sync}
        self.sems = {}
        self.cnt = {}
        self.seen = {e: {} for e in self.eng}
        self.dry = False
        self.loopvars = {}
        self.nvars = 0
        self.bufs = []
        self.serial = 0
        self.pending = {e: [] for e in self.eng}
        self.dmakeys = {}
        self.free_dsems = []
        self.ninstr = 0
        for e in self.eng:
            self._mksem("E_" + e)
        for i in range(self.NDMASEM):
            k = "D_%d" % i
            self._mksem(k)
            self.free_dsems.append(k)
        for k, s in self.sems.items():
            left = SEM_BIAS
            while left > 0:
                step = min(left, 10000)
                nc.sync.sem_inc(s, step)
                left -= step
            self.cnt[k] = LE(SEM_BIAS)

    def _mksem(self, key):
        self.sems[key] = self.es.enter_context(self.nc.semaphore(key))
        self.cnt[key] = LE(0)

    def buf(self, name):
        return Buf(self, name)

    def bufs_n(self, name, n):
        return [Buf(self, "%s%d" % (name, i)) for i in range(n)]

    def val(self, le):
        v = le.c
        for var, coef in le.t.items():
            v = self.loopvars[var] * coef + v
        return v

    def _wait(self, e, tok):
        key, le, _ = tok
        if key == "E_pe" and e == "pe":
            return
        s = self.seen[e].get(key)
        if s is not None and s.same(le) and s.c >= le.c:
            return
        if not self.dry:
            self.eng[e].wait_ge(self.sems[key], self.val(le))
            self.ninstr += 1
        if s is None or not s.same(le) or s.c < le.c:
            self.seen[e][key] = le

    def _deps(self, reads, writes):
        deps = []
        for b in reads:
            deps += b.ws
        for b in writes:
            deps += b.ws
            deps += b.rs
        return deps

    def _commit(self, tok, reads, writes):
        for b in reads:
            b.rs = _prune(b.rs + [tok])
        for b in writes:
            b.ws = [tok]
            b.rs = []

    def op(self, e, fn, reads=(), writes=(), signal=True):
        for tok in self._deps(reads, writes):
            self._wait(e, tok)
        ins = None
        if not self.dry:
            ins = fn()
            self.ninstr += 1
        if signal:
            key = "E_" + e
            self.cnt[key] = self.cnt[key].add(1)
            if not self.dry:
                ins.then_inc(self.sems[key], 1)
            self.serial += 1
            tok = (key, self.cnt[key], self.serial)
            for (r, w) in self.pending[e]:
                self._commit(tok, r, w)
            self.pending[e] = []
            self._commit(tok, reads, writes)
        else:
            self.pending[e].append((tuple(reads), tuple(writes)))

    def dma(self, q, pairs, reads=(), writes=(), key=None, **kw):
        if key is None:
            key = "_anon_%s" % q
        if key not in self.dmakeys:
            sk = self.free_dsems.pop(0)
            self.dmakeys[key] = (sk, Buf(self, "dsem_" + key))
        sk, sbuf = self.dmakeys[key]
        for tok in self._deps(reads, tuple(writes) + (sbuf,)):
            self._wait(q, tok)
        if callable(pairs):
            pairs = pairs() if not self.dry else [None] * pairs.n
        n = len(pairs)
        if not self.dry:
            for (o, i) in pairs:
                self.eng[q].dma_start(out=o, in_=i, **kw).then_inc(self.sems[sk], 16)
                self.ninstr += 1
        self.cnt[sk] = self.cnt[sk].add(16 * n)
        self.serial += 1
        tok = (sk, self.cnt[sk], self.serial)
        self._commit(tok, reads, tuple(writes) + (sbuf,))

    def dma_raw(self, q, fn, reads=(), writes=(), key=None):
        if key not in self.dmakeys:
            sk = self.free_dsems.pop(0)
            self.dmakeys[key] = (sk, Buf(self, "dsem_" + key))
        sk, sbuf = self.dmakeys[key]
        for tok in self._deps(reads, tuple(writes) + (sbuf,)):
            self._wait(q, tok)
        if not self.dry:
            fn().then_inc(self.sems[sk], 16)
            self.ninstr += 1
        self.cnt[sk] = self.cnt[sk].add(16)
        self.serial += 1
        tok = (sk, self.cnt[sk], self.serial)
        self._commit(tok, reads, tuple(writes) + (sbuf,))

    def _snapshot(self):
        return (
            {id(b): (list(b.ws), list(b.rs)) for b in self.bufs},
            dict(self.cnt),
            {e: dict(m) for e, m in self.seen.items()},
            {e: list(p) for e, p in self.pending.items()},
            dict(self.dmakeys),
            list(self.free_dsems),
            len(self.bufs),
        )

    def _restore(self, snap):
        bs, cnt, seen, pend, dk, fd, nb = snap
        del self.bufs[nb:]
        for b in self.bufs:
            b.ws, b.rs = list(bs[id(b)][0]), list(bs[id(b)][1])
        self.cnt = dict(cnt)
        self.seen = {e: dict(m) for e, m in seen.items()}
        self.pending = {e: list(p) for e, p in pend.items()}
        self.dmakeys = dict(dk)
        self.free_dsems = list(fd)

    def loop(self, trips, body):
        if trips == 1:
            body(0)
            return
        for e in self.eng:
            assert not self.pending[e], "unsignalled ops pending at loop entry"
        snap = self._snapshot()
        serial0 = self.serial
        entry = dict(self.cnt)
        was_dry = self.dry
        self.dry = True
        body(0)
        for e in self.eng:
            assert not self.pending[e], "unsignalled ops pending at loop end"
        n = {k: self.cnt[k].c - entry[k].c for k in self.cnt}
        new_keys = dict(self.dmakeys)
        new_free = list(self.free_dsems)
        end = {id(b): (list(b.ws), list(b.rs)) for b in self.bufs}
        allbufs = list(self.bufs)
        self._restore(snap)
        self.bufs = allbufs
        self.dmakeys = new_keys
        self.free_dsems = new_free
        for b in self.bufs:
            if id(b) not in snap[0]:
                b.ws, b.rs = [], []
        self.dry = was_dry
        if self.dry:
            for k in self.cnt:
                self.cnt[k] = entry[k].add(trips * n[k])
            for b in self.bufs:
                ws, rs = end[id(b)]
                b.ws = [(k, le.add((trips - 1) * n[k]), s) if s > serial0 else (k, le, s) for (k, le, s) in ws]
                b.rs = [(k, le.add((trips - 1) * n[k]), s) if s > serial0 else (k, le, s) for (k, le, s) in rs]
            return
        vid = self.nvars
        self.nvars += 1
        with self.nc.Fori(0, trips) as iv:
            self.loopvars[vid] = iv
            for k in self.cnt:
                if n[k]:
                    self.cnt[k] = entry[k].addvar(vid, n[k])
            for b in self.bufs:
                ws, rs = end[id(b)]
                cw = [(k, le.add(-n[k]).addvar(vid, n[k]), s) for (k, le, s) in ws if s > serial0]
                cr = [(k, le.add(-n[k]).addvar(vid, n[k]), s) for (k, le, s) in rs if s > serial0]
                b.ws = _prune(b.ws + cw)
                b.rs = _prune(b.rs + cr)
            self.seen = {e: {} for e in self.eng}
            body(iv)
        del self.loopvars[vid]
        for k in self.cnt:
            self.cnt[k] = entry[k].add(trips * n[k])
        for b in self.bufs:
            b.ws = _prune([(k, le.subst(vid, trips - 1), s) for (k, le, s) in b.ws])
            b.rs = _prune([(k, le.subst(vid, trips - 1), s) for (k, le, s) in b.rs])
        self.seen = {e: {} for e in self.eng}

    def finish(self, e="sp"):
        for k in self.cnt:
            self._wait(e, (k, self.cnt[k], 0))

    def barrier(self):
        last = getattr(self, "_bar_cnt", {})
        for e in self.eng:
            for k in self.cnt:
                le = self.cnt[k]
                if k in last and last[k].same(le) and last[k].c == le.c:
                    continue
                self._wait(e, (k, le, 0))
        self._bar_cnt = dict(self.cnt)
        for key, (sk, b) in self.dmakeys.items():
            self.free_dsems.append(sk)
        self.dmakeys = {}


D = 2048
T = 4096
NTB = T // 512
NTT = T // 128
DEPTH = 2
H = 16
OFF_Q, OFF_KV, OFF_KR, OFF_U, OFF_V, OFF_F, OFF_G, N_IN = 0, 512, 1024, 1088, 3136, 5184, 7232, 13376
ALPHA = (2 * DEPTH) ** 0.25
MLA_SCALE = 192 ** -0.5
NEXP = 32
CAP = 768
MEM = 256


class Prog:
    def __init__(self, cfg=None):
        self.cfg = cfg or {}
        self.nc = bass.Bass("TRN2", target_bir_lowering=False)
        self.es = ExitStack()
        self.dram = {}
        self.expose_in = set(self.cfg.get("expose_in", ()))
        self.expose_out = set(self.cfg.get("expose_out", ()))

    def dt(self, name, shape, dtype, kind=None):
        if kind is None:
            kind = "Internal"
            if name in self.expose_in:
                kind = "ExternalInput"
            elif name in self.expose_out:
                kind = "ExternalOutput"
        t = self.nc.dram_tensor(name, list(shape), dtype, kind=kind).ap()
        self.dram[name] = (t, kind, tuple(shape), dtype)
        return t

    def sb(self, es, name, shape, dtype):
        self._nsb = getattr(self, "_nsb", 0) + 1
        return es.enter_context(self.nc.sbuf_tensor("%s_%d" % (name, self._nsb), list(shape), dtype))


def _ceil(a, b):
    return (a + b - 1) // b


class Gemm:
    def __init__(self, P, S, es, KC, pw=512, nslots=2, wname="wp"):
        self.P, self.S = P, S
        nc = P.nc
        self.KC, self.pw = KC, pw
        self.wp = [P.sb(es, "%s%d" % (wname, i), [128, KC, pw], BF16) for i in range(nslots)]
        self.b_wp = S.bufs_n(wname, nslots)
        self.nslots = nslots
        self.slot = 0

    def run(self, A, bA, W, N, orient, epi, Tc=T, w_is_f32=True, unroll=False, acols=None):
        P, S, nc = self.P, self.S, self.P.nc
        KC, pw = self.KC, self.pw
        wv = W.rearrange("(c p) n -> p c n", p=128)
        wq = "pool" if w_is_f32 else "sp"
        full, rem = N // pw, N % pw

        def panel(pi, width, slot):
            wp = self.wp[slot]
            S.dma(wq, [(wp[:, :, 0:width], wv[:, :, bass.ds(pi * pw, width)])], writes=[self.b_wp[slot]],
                  key="wp%d" % slot)
            if orient == "fm":
                for sub in range(_ceil(width, 128)):
                    m = min(128, width - sub * 128)
                    for tb in range(Tc // 512):
                        q = (sub * (Tc // 512) + tb) % P.NGPS
                        for k in range(KC):
                            S.op("pe", lambda k=k, q=q, tb=tb, m=m, sub=sub: nc.tensor.matmul(
                                P.ps[q][0:m, :], wp[:, k, sub * 128:sub * 128 + m], A[:, k, tb * 512:(tb + 1) * 512],
                                start=(k == 0), stop=(k == KC - 1)),
                                reads=[self.b_wp[slot], bA], writes=[P.b_ps[q]], signal=(k == KC - 1))
                        epi(q, m, pi * (pw // 128) + sub, tb, 512)
            else:
                for tt in range(Tc // 128):
                    q = tt % P.NGPS
                    for k in range(KC):
                        S.op("pe", lambda k=k, q=q, tt=tt: nc.tensor.matmul(
                            P.ps[q][:, 0:width], A[:, k, tt * 128:(tt + 1) * 128], wp[:, k, 0:width],
                            start=(k == 0), stop=(k == KC - 1)),
                            reads=[self.b_wp[slot], bA], writes=[P.b_ps[q]], signal=(k == KC - 1))
                    epi(q, 128, pi * pw, tt, width)

        ns = self.nslots
        if USE_LOOPS and full >= 2 * ns and not unroll and full % ns == 0:
            def body(i):
                for s in range(ns):
                    panel(i * ns + s, pw, s)
            S.loop(full // ns, body)
        else:
            for pi in range(full):
                panel(pi, pw, pi % ns)
        if rem:
            panel(full, rem, full % ns)


def build_program(cfg=None):
    cfg = cfg or {}
    P = Prog(cfg)
    nc = P.nc
    es = P.es
    S = Sched(nc, es)
    P.S = S
    layers = cfg.get("layers", list(range(DEPTH)))
    phases = cfg.get("phases", None)

    def want(name):
        return phases is None or name in phases

    EI = "ExternalInput"
    x_in = P.dt("x_in", [T, D], F32, EI)
    w_in = P.dt("w_in", [DEPTH, D, N_IN], F32, EI)
    b_gate = P.dt("b_gate", [DEPTH, 3 * D, 1], F32, EI)
    ident_d = P.dt("ident", [128, 128], BF16, EI)
    cT = P.dt("cT", [1088, T], F32)
    uT = P.dt("uT", [D, T], BF16)
    vtok = P.dt("vtok", [T, D], BF16)
    fT = P.dt("fT", [D, T], BF16)
    gT = P.dt("gT", [3 * D, T], BF16)

    P.NGPS = 4
    P.pst = es.enter_context(nc.psum_tensor("pst", [128, 8, 512], F32))
    P.ps = [P.pst[:, i, :] for i in range(8)]
    P.b_ps = S.bufs_n("ps", 8)
    ident = P.sb(es, "ident_sb", [128, 128], BF16)
    b_const = S.buf("const")
    S.dma("sp", [(ident[:], ident_d[:, :])], writes=[b_const], key="const")

    def stage_out(pes, name, n, shape, dtype):
        tiles = [P.sb(pes, "%s%d" % (name, i), shape, dtype) for i in range(n)]
        return tiles, S.bufs_n(name, n)


    def build_xT(xT, b_xT, xs, b_xs, x_src, ntok):
        for tt in range(ntok // 128):
            s = tt % 2
            xrow = xs[s][:, 0:4, :].rearrange("p a b -> p (a b)")
            S.dma("pool", [(xrow, x_src[tt * 128:(tt + 1) * 128, :])], writes=[b_xs[s]], key="wp%d" % s)
            for half in range(2):
                q = 4 + (tt * 2 + half) % 2
                pst = P.ps[q].bitcast(BF16)
                for j in range(8):
                    c = half * 8 + j
                    S.op("pe", lambda c=c, j=j, pst=pst, xrow=xrow: nc.tensor.transpose(
                        pst[:, j * 128:(j + 1) * 128], xrow[:, c * 128:(c + 1) * 128], ident[:]),
                        reads=[b_xs[s], b_const], writes=[P.b_ps[q]], signal=(j == 7))
                dst = xT[:, half * 8:(half + 1) * 8, tt * 128:(tt + 1) * 128]
                src = pst.rearrange("p (j t) -> p j t", j=8)
                if half == 0:
                    S.op("act", lambda dst=dst, src=src: nc.scalar.activation(out=dst, in_=src, func=AF.Copy),
                         reads=[P.b_ps[q]], writes=[b_xT])
                else:
                    S.op("dve", lambda dst=dst, src=src: nc.vector.tensor_copy(out=dst, in_=src),
                         reads=[P.b_ps[q]], writes=[b_xT])

    def phase_zgemm(l, x_src):
        with ExitStack() as pes:
            xT = P.sb(pes, "xT", [128, 16, T], BF16)
            b_xT = S.buf("xT")
            G = Gemm(P, S, pes, 16)
            build_xT(xT, b_xT, G.wp, G.b_wp, x_src, T)
            o32, b_o32 = stage_out(pes, "o32_", 2, [128, 512], F32)
            o16, b_o16 = stage_out(pes, "o16_", 4, [128, 512], BF16)
            bg, b_bg = stage_out(pes, "bg_", 2, [128, 1], F32)
            st = {"i": 0}

            def epi_lat(q, m, n0, tb, width):
                j = (tb) % 2
                S.op("act", lambda: nc.scalar.activation(out=o32[j][0:m, :], in_=P.ps[q][0:m, :], func=AF.Copy),
                     reads=[P.b_ps[q]], writes=[b_o32[j]])
                S.dma("sp", [(cT[n0 * 128:n0 * 128 + m, tb * 512:(tb + 1) * 512], o32[j][0:m, :])], reads=[b_o32[j]],
                      key="st32_%d" % j)

            def mk_epi_fm(dst, func, bias_src=None):
                def epi(q, m, n0, tb, width):
                    j = tb % 4
                    if bias_src is not None and tb == 0:
                        S.dma("sp", [(bg[0][:, :], bias_src.rearrange("(j p) o -> j p o", p=128)[n0])], writes=[b_bg[0]], key="bg")
                    if bias_src is not None:
                        S.op("act", lambda: nc.scalar.activation(out=o16[j][:, :], in_=P.ps[q][:, :], func=func,
                                                                 bias=bg[0][:, 0:1]),
                             reads=[P.b_ps[q], b_bg[0]], writes=[b_o16[j]])
                    else:
                        S.op("act", lambda: nc.scalar.activation(out=o16[j][:, :], in_=P.ps[q][:, :], func=func),
                             reads=[P.b_ps[q]], writes=[b_o16[j]])
                    S.dma("sp", [(dst.rearrange("(j p) t -> j p t", p=128)[n0][:, tb * 512:(tb + 1) * 512], o16[j][:, :])], reads=[b_o16[j]],
                          key="st16_%d" % j)
                return epi

            def epi_v(q, m, n0, tt, width):
                j = tt % 4
                S.op("act", lambda: nc.scalar.activation(out=o16[j][:, :], in_=P.ps[q][:, :], func=AF.Gelu),
                     reads=[P.b_ps[q]], writes=[b_o16[j]])
                S.dma("sp", [(vtok[tt * 128:(tt + 1) * 128, bass.ds(n0, 512)], o16[j][:, :])], reads=[b_o16[j]],
                      key="st16_%d" % j)

            W = w_in[l]
            G.run(xT, b_xT, W[:, 0:OFF_U], OFF_U, "fm", epi_lat)
            G.run(xT, b_xT, W[:, OFF_U:OFF_V], D, "fm", mk_epi_fm(uT, AF.Gelu))
            G.run(xT, b_xT, W[:, OFF_V:OFF_F], D, "tm", epi_v)
            G.run(xT, b_xT, W[:, OFF_F:OFF_G], D, "fm", mk_epi_fm(fT, AF.Copy))
            G.run(xT, b_xT, W[:, OFF_G:N_IN], 3 * D, "fm", mk_epi_fm(gT, AF.Sigmoid, bias_src=b_gate[l]))
            S.barrier()


    qnorm_d = P.dt("mla_q_norm", [DEPTH, 128, 4], F32, EI)
    kvnorm_d = P.dt("mla_kv_norm", [DEPTH, 128, 4], F32, EI)
    w_uq = P.dt("w_uq", [DEPTH, 512, 3072], F32, EI)
    w_ukv = P.dt("w_ukv", [DEPTH, 512, 4096], F32, EI)
    rope_cs = P.dt("rope_cs", [2, 64, T], F32, EI)
    qT = P.dt("qT", [H, 192, T], BF16)
    kT = P.dt("kT", [H, 128, T], BF16)
    krT = P.dt("krT", [64, T], BF16)
    vtm = P.dt("vtm", [T, D], BF16)
    ones = P.sb(es, "ones_sb", [128, 128], BF16)
    S.op("dve", lambda: nc.vector.memset(ones[:], 1.0), writes=[b_const])

    def evac(i, out, in_, reads, writes, func=None, **kw):
        if i % 2 == 0:
            S.op("act", lambda: nc.scalar.activation(out=out, in_=in_, func=AF.Copy), reads=reads, writes=writes)
        else:
            S.op("dve", lambda: nc.vector.tensor_copy(out=out, in_=in_), reads=reads, writes=writes)

    def phase_mla_prep(l):
        with ExitStack() as pes:
            wq = P.sb(pes, "wq_all", [128, 4, 3072], BF16)
            wqs = P.sb(pes, "wq_s", [128, 4, 16, 64], BF16)
            wkc = P.sb(pes, "wk_c", [128, 4, 2048], BF16)
            wvc = P.sb(pes, "wv_c", [128, 4, 2048], BF16)
            gq = P.sb(pes, "gq", [128, 8], F32)
            b_w = S.buf("mlaw")
            wkv_v = w_ukv[l].rearrange("(c p) (h e) -> p c h e", p=128, e=256)
            S.dma("pool", [(wq[:], w_uq[l].rearrange("(c p) n -> p c n", p=128))]
                  + [(wkc[:, c, :].rearrange("p (h e) -> p h e", e=128), wkv_v[:, c, :, 0:128]) for c in range(4)]
                  + [(wvc[:, c, :].rearrange("p (h e) -> p h e", e=128), wkv_v[:, c, :, 128:256]) for c in range(4)],
                  writes=[b_w], key="mlaw")
            S.dma("sp", [(gq[:, 0:4], qnorm_d[l]), (gq[:, 4:8], kvnorm_d[l])], writes=[b_w], key="mlag")
            wq4 = wq[:].rearrange("p c (h e) -> p c h e", e=192)
            S.op("dve", lambda: nc.vector.tensor_copy(out=wqs[:, :, :, 0:32], in_=wq4[:, :, :, 160:192]), reads=[b_w], writes=[b_w])
            S.op("dve", lambda: nc.vector.tensor_copy(out=wqs[:, :, :, 32:64], in_=wq4[:, :, :, 128:160]), reads=[b_w], writes=[b_w])
            NB = 2
            cb = [P.sb(pes, "cb%d" % i, [128, 9, 512], F32) for i in range(NB)]
            cbs = [P.sb(pes, "cbs%d" % i, [64, 512], F32) for i in range(NB)]
            cs = [P.sb(pes, "cs%d" % i, [64, 2, 512], F32) for i in range(NB)]
            b_cb = S.bufs_n("cb", NB)
            sq = P.sb(pes, "sq", [128, 8, 512], BF16)
            b_sq = S.buf("sq")
            rs = P.sb(pes, "rs", [128, 2, 512], F32)
            b_rs = S.buf("rs")
            cn = P.sb(pes, "cn", [128, 8, 512], BF16)
            b_cn = S.buf("cn")
            st_n = [P.sb(pes, "st_n%d" % i, [128, 16, 512], BF16) for i in range(2)]
            b_stn = S.bufs_n("stn", 2)
            st_r = P.sb(pes, "st_r", [64, 16, 512], BF16)
            b_str = S.buf("str")
            st_v = P.sb(pes, "st_v", [128, 4, 2048], BF16)
            b_stv = S.buf("stv")
            st_kr = P.sb(pes, "st_kr", [64, 512], BF16)
            b_stkr = S.buf("stkr")
            t1 = [P.sb(pes, "t1_%d" % i, [64, 512], F32) for i in range(2)]
            t2 = [P.sb(pes, "t2_%d" % i, [64, 512], F32) for i in range(2)]
            b_t = S.bufs_n("t12", 2)
            cv = cT.rearrange("(c p) t -> p c t", p=128) if False else None
            for tb in range(NTB):
                s = tb % NB
                tsl = slice(tb * 512, (tb + 1) * 512)
                S.dma("sp", [(cb[s][:, 0:8, :], cT[0:1024, tsl].rearrange("(c p) t -> p c t", p=128)),
                             (cb[s][0:64, 8, :], cT[1024:1088, tsl]),
                             (cbs[s][0:32, :], cT[1056:1088, tsl]),
                             (cbs[s][32:64, :], cT[1024:1056, tsl]),
                             (cs[s][:, 0, :], rope_cs[0][:, tsl]),
                             (cs[s][:, 1, :], rope_cs[1][:, tsl])], writes=[b_cb[s]], key="cb%d" % s)
                S.op("act", lambda s=s: nc.scalar.activation(out=sq[:], in_=cb[s][:, 0:8, :], func=AF.Square),
                     reads=[b_cb[s]], writes=[b_sq])
                for g in range(2):
                    q = 6 + g
                    for k in range(4):
                        S.op("pe", lambda g=g, k=k, q=q: nc.tensor.matmul(P.ps[q][:], ones[:], sq[:, g * 4 + k, :], start=(k == 0), stop=(k == 3)),
                             reads=[b_sq, b_const], writes=[P.b_ps[q]], signal=(k == 3))
                    S.op("act", lambda g=g, q=q: nc.scalar.activation(out=rs[:, g, :], in_=P.ps[q][:], func=AF.Sqrt, scale=1.0 / 512, bias=1e-6),
                         reads=[P.b_ps[q]], writes=[b_rs])
                S.op("dve", lambda: nc.vector.reciprocal(out=rs[:], in_=rs[:]), reads=[b_rs], writes=[b_rs])
                for k in range(8):
                    S.op("dve", lambda k=k, s=s: nc.vector.scalar_tensor_tensor(out=cn[:, k, :], in0=cb[s][:, k, :], scalar=gq[:, k:k + 1],
                                                                          in1=rs[:, k // 4, :], op0=ALU.mult, op1=ALU.mult),
                         reads=[b_cb[s], b_rs, b_w], writes=[b_cn])
                sn = st_n[0]
                for h in range(H):
                    q = h % 2
                    for k in range(4):
                        S.op("pe", lambda h=h, k=k, q=q: nc.tensor.matmul(P.ps[q][:], wq[:, k, h * 192:h * 192 + 128], cn[:, k, :], start=(k == 0), stop=(k == 3)),
                             reads=[b_w, b_cn], writes=[P.b_ps[q]], signal=(k == 3))
                    evac(h, sn[:, h, :], P.ps[q][:], [P.b_ps[q]], [b_stn[0]])
                    qa, qb = 2 + (h % 2) * 2, 3 + (h % 2) * 2
                    for k in range(4):
                        S.op("pe", lambda h=h, k=k, qa=qa: nc.tensor.matmul(P.ps[qa][0:64, :], wq[:, k, h * 192 + 128:h * 192 + 192], cn[:, k, :], start=(k == 0), stop=(k == 3)),
                             reads=[b_w, b_cn], writes=[P.b_ps[qa]], signal=(k == 3))
                    for k in range(4):
                        S.op("pe", lambda h=h, k=k, qb=qb: nc.tensor.matmul(P.ps[qb][0:64, :], wqs[:, k, h, :], cn[:, k, :], start=(k == 0), stop=(k == 3)),
                             reads=[b_w, b_cn], writes=[P.b_ps[qb]], signal=(k == 3))
                    j = h % 2
                    S.op("dve", lambda j=j, qa=qa, s=s: nc.vector.tensor_tensor(out=t1[j][:], in0=P.ps[qa][0:64, :], in1=cs[s][:, 0, :], op=ALU.mult),
                         reads=[P.b_ps[qa], b_cb[s]], writes=[b_t[j]])
                    S.op("dve", lambda j=j, qb=qb, s=s: nc.vector.tensor_tensor(out=t2[j][:], in0=P.ps[qb][0:64, :], in1=cs[s][:, 1, :], op=ALU.mult),
                         reads=[P.b_ps[qb], b_cb[s]], writes=[b_t[j]])
                    S.op("dve", lambda j=j, h=h: nc.vector.tensor_tensor(out=st_r[:, h, :], in0=t1[j][:], in1=t2[j][:], op=ALU.add),
                         reads=[b_t[j]], writes=[b_str])
                S.dma("sp", [(qT[:, 0:128, tsl].rearrange("h p t -> p h t"), sn[:]),
                             (qT[:, 128:192, tsl].rearrange("h p t -> p h t"), st_r[:])], reads=[b_stn[0], b_str], key="stq")
                sk = st_n[1]
                for h in range(H):
                    q = h % 2
                    for k in range(4):
                        S.op("pe", lambda h=h, k=k, q=q: nc.tensor.matmul(P.ps[q][:], wkc[:, k, h * 128:h * 128 + 128], cn[:, 4 + k, :], start=(k == 0), stop=(k == 3)),
                             reads=[b_w, b_cn], writes=[P.b_ps[q]], signal=(k == 3))
                    evac(h + 1, sk[:, h, :], P.ps[q][:], [P.b_ps[q]], [b_stn[1]])
                S.dma("sp", [(kT[:, :, tsl].rearrange("h p t -> p h t"), sk[:])], reads=[b_stn[1]], key="stk")
                for tt in range(4):
                    for cp in range(4):
                        q = 2 + (tt * 4 + cp) % 4
                        for k in range(4):
                            S.op("pe", lambda tt=tt, cp=cp, k=k, q=q: nc.tensor.matmul(P.ps[q][:], cn[:, 4 + k, tt * 128:(tt + 1) * 128], wvc[:, k, cp * 512:(cp + 1) * 512],
                                                                                 start=(k == 0), stop=(k == 3)),
                                 reads=[b_w, b_cn], writes=[P.b_ps[q]], signal=(k == 3))
                        evac(tt * 4 + cp, st_v[:, tt, cp * 512:(cp + 1) * 512], P.ps[q][:], [P.b_ps[q]], [b_stv])
                S.dma("sp", [(vtm[tsl, :].rearrange("(a p) n -> p a n", p=128), st_v[:])], reads=[b_stv], key="stv")
                S.op("dve", lambda s=s: nc.vector.tensor_tensor(out=t1[0][:], in0=cb[s][0:64, 8, :], in1=cs[s][:, 0, :], op=ALU.mult),
                     reads=[b_cb[s]], writes=[b_t[0]])
                S.op("dve", lambda s=s: nc.vector.tensor_tensor(out=t2[0][:], in0=cbs[s][:], in1=cs[s][:, 1, :], op=ALU.mult),
                     reads=[b_cb[s]], writes=[b_t[0]])
                S.op("dve", lambda: nc.vector.tensor_tensor(out=st_kr[:], in0=t1[0][:], in1=t2[0][:], op=ALU.add),
                     reads=[b_t[0]], writes=[b_stkr])
                S.dma("sp", [(krT[:, tsl], st_kr[:])], reads=[b_stkr], key="stkr")
            S.barrier()


    amask_d = P.dt("attn_mask", [128, NTT * NTB], F32, EI)
    oT = P.dt("oT", [D, T], BF16)

    def phase_attn(l):
        with ExitStack() as pes:
            NB = 2
            QA = [P.sb(pes, "QA%d" % i, [128, T], BF16) for i in range(NB)]
            QB = [P.sb(pes, "QB%d" % i, [128, T], BF16) for i in range(NB)]
            KA = [P.sb(pes, "KA%d" % i, [128, T], BF16) for i in range(NB)]
            V = [P.sb(pes, "V%d" % i, [128, NTT, 128], BF16) for i in range(NB)]
            b_hd = S.bufs_n("hd", NB)
            KB = P.sb(pes, "KB", [128, T], BF16)
            b_kb = S.buf("KB")
            sqA = P.sb(pes, "sqA", [128, T], BF16)
            sqB = P.sb(pes, "sqB", [128, T], BF16)
            b_sq = S.buf("asq")
            mask = P.sb(pes, "amask", [128, NTT * NTB], F32)
            biash = P.sb(pes, "biash", [128, NTT * NTB], F32)
            b_bias = S.buf("biash")
            mx = P.sb(pes, "mx", [128, 32], F32)
            b_mx = S.buf("mx")
            Pt = [P.sb(pes, "Pt%d" % i, [128, 2, 512], BF16) for i in range(4)]
            b_pt = S.bufs_n("Pt", 4)
            b_pp = S.bufs_n("pspair", 3)
            acc = [P.sb(pes, "acc%d" % i, [128, 2, 512], BF16) for i in range(2)]
            accp = [P.sb(pes, "accp%d" % i, [128, 2, 512], BF16) for i in range(2)]
            accb = [P.sb(pes, "accb%d" % i, [128, 2, 512], BF16) for i in range(2)]
            b_acc = S.bufs_n("acc", 2)
            b_accp = S.bufs_n("accp", 2)
            b_accb = S.bufs_n("accb", 2)
            rinv = [P.sb(pes, "rinv%d" % i, [128, 512], F32) for i in range(2)]
            b_rinv = S.bufs_n("rinv", 2)
            osb = [P.sb(pes, "osb%d" % i, [128, T], BF16) for i in range(2)]
            b_osb = S.bufs_n("osb", 2)
            S.op("dve", lambda: nc.vector.memset(KB[64:128, :], 0.0), writes=[b_kb])
            for i in range(NB):
                S.op("dve", lambda i=i: nc.vector.memset(QB[i][64:128, :], 0.0), writes=[b_hd[i]])
            S.dma("sp", [(KB[0:64, :], krT[:, :]), (mask[:], amask_d[:, :])], writes=[b_kb], key="kb")
            S.op("act", lambda: nc.scalar.activation(out=sqB[:], in_=KB[:], func=AF.Square), reads=[b_kb], writes=[b_sq])
            for c in range(NTB):
                q = 6 + c % 2
                S.op("pe", lambda c=c, q=q: nc.tensor.matmul(P.ps[q][:], ones[:], sqB[:, c * 512:(c + 1) * 512], start=True, stop=True),
                     reads=[b_sq, b_const], writes=[P.b_ps[q]])
                S.op("dve", lambda c=c, q=q: nc.vector.reduce_max(out=mx[:, 8 + c:9 + c], in_=P.ps[q][:], axis=AX.X), reads=[P.b_ps[q]], writes=[b_mx])
            S.op("dve", lambda: nc.vector.reduce_max(out=mx[:, 24:25], in_=mx[:, 8:16], axis=AX.X), reads=[b_mx], writes=[b_mx])

            def load_head(h):
                s = h % NB
                vv = vtm[:, h * 128:(h + 1) * 128].rearrange("(a p) d -> p a d", p=128)
                S.dma("sp", [(QA[s][:], qT[h][0:128, :]), (QB[s][0:64, :], qT[h][128:192, :]), (KA[s][:], kT[h])]
                      + [(V[s][:, a * 8:(a + 1) * 8, :], vv[:, a * 8:(a + 1) * 8, :]) for a in range(4)],
                      writes=[b_hd[s]], key="hd%d" % s)

            load_head(0)
            for h in range(H):
                s = h % NB
                if h + 1 < H:
                    load_head(h + 1)
                S.op("act", lambda s=s: nc.scalar.activation(out=sqA[:], in_=QA[s][:], func=AF.Square), reads=[b_hd[s]], writes=[b_sq])
                S.op("act", lambda s=s: nc.scalar.activation(out=sqB[:], in_=QB[s][:], func=AF.Square), reads=[b_hd[s]], writes=[b_sq])
                for c in range(NTB):
                    q = 6 + c % 2
                    S.op("pe", lambda c=c, q=q: nc.tensor.matmul(P.ps[q][:], ones[:], sqA[:, c * 512:(c + 1) * 512], start=True, stop=False),
                         reads=[b_sq, b_const], writes=[P.b_ps[q]], signal=False)
                    S.op("pe", lambda c=c, q=q: nc.tensor.matmul(P.ps[q][:], ones[:], sqB[:, c * 512:(c + 1) * 512], start=False, stop=True),
                         reads=[b_sq, b_const], writes=[P.b_ps[q]])
                    S.op("dve", lambda c=c, q=q: nc.vector.reduce_max(out=mx[:, c:c + 1], in_=P.ps[q][:], axis=AX.X), reads=[P.b_ps[q]], writes=[b_mx])
                S.op("act", lambda s=s: nc.scalar.activation(out=sqA[:], in_=KA[s][:], func=AF.Square), reads=[b_hd[s], b_sq], writes=[b_sq])
                for c in range(NTB):
                    q = 6 + c % 2
                    S.op("pe", lambda c=c, q=q: nc.tensor.matmul(P.ps[q][:], ones[:], sqA[:, c * 512:(c + 1) * 512], start=True, stop=True),
                         reads=[b_sq, b_const], writes=[P.b_ps[q]])
                    S.op("dve", lambda c=c, q=q: nc.vector.reduce_max(out=mx[:, 8 + c:9 + c], in_=P.ps[q][:], axis=AX.X), reads=[P.b_ps[q]], writes=[b_mx])
                S.op("dve", lambda: nc.vector.reduce_max(out=mx[:, 16:17], in_=mx[:, 0:8], axis=AX.X), reads=[b_mx], writes=[b_mx])
                S.op("dve", lambda: nc.vector.reduce_max(out=mx[:, 17:18], in_=mx[:, 8:16], axis=AX.X), reads=[b_mx], writes=[b_mx])
                S.op("dve", lambda: nc.vector.tensor_tensor(out=mx[:, 17:18], in0=mx[:, 17:18], in1=mx[:, 24:25], op=ALU.add), reads=[b_mx], writes=[b_mx])
                S.op("dve", lambda: nc.vector.tensor_tensor(out=mx[:, 18:19], in0=mx[:, 16:17], in1=mx[:, 17:18], op=ALU.mult), reads=[b_mx], writes=[b_mx])
                S.op("act", lambda: nc.scalar.activation(out=mx[:, 19:20], in_=mx[:, 18:19], func=AF.Sqrt, scale=MLA_SCALE * MLA_SCALE), reads=[b_mx], writes=[b_mx])
                S.op("dve", lambda: nc.vector.tensor_scalar(out=biash[:], in0=mask[:], scalar1=mx[:, 19:20], scalar2=None, op0=ALU.subtract),
                     reads=[b_mx, b_kb], writes=[b_bias])
                for qc in range(NTB):
                    j = qc % 2
                    qo = 6 + j
                    qs = slice(qc * 512, (qc + 1) * 512)
                    def unit_s(kp):
                        pq = kp % 3
                        pj = kp % 4
                        for t in range(2):
                            kt = kp * 2 + t
                            ks = slice(kt * 128, (kt + 1) * 128)
                            S.op("pe", lambda t=t, ks=ks: nc.tensor.matmul(P.pst[:, pq * 2 + t, :], KA[s][:, ks], QA[s][:, qs], start=True, stop=False),
                                 reads=[b_hd[s]], writes=[b_pp[pq]], signal=False)
                            S.op("pe", lambda t=t, ks=ks: nc.tensor.matmul(P.pst[:, pq * 2 + t, :], KB[:, ks], QB[s][:, qs], start=False, stop=True),
                                 reads=[b_hd[s], b_kb], writes=[b_pp[pq]], signal=(t == 1))
                        bcol = kp * 2 * NTB + qc
                        S.op("act", lambda: nc.scalar.activation(out=Pt[pj][:], in_=P.pst[:, pq * 2:pq * 2 + 2, :], func=AF.Exp, scale=MLA_SCALE,
                                                                 bias=biash[:, bcol:bcol + 1]),
                             reads=[b_pp[pq], b_bias], writes=[b_pt[pj]])

                    def unit_pv(kp):
                        pj = kp % 4
                        for t in range(2):
                            kt = kp * 2 + t
                            S.op("pe", lambda t=t, kt=kt: nc.tensor.matmul(P.ps[qo], V[s][:, kt, :], Pt[pj][:, t, :], start=(kt == 0), stop=(kt == NTT - 1)),
                                 reads=[b_hd[s], b_pt[pj]], writes=[P.b_ps[qo]], signal=(t == 1))
                        if kp % 2 == 0:
                            a, ba = acc[j], b_acc[j]
                        else:
                            a, ba = accp[j], b_accp[j]
                        if kp < 2:
                            S.op("dve", lambda: nc.vector.tensor_copy(out=a[:], in_=Pt[pj][:]), reads=[b_pt[pj]], writes=[ba])
                        else:
                            S.op("dve", lambda: nc.vector.tensor_tensor(out=a[:], in0=a[:], in1=Pt[pj][:], op=ALU.add), reads=[b_pt[pj]], writes=[ba])

                    LAG = 2
                    NKP = NTT // 2
                    for kp in range(NKP + LAG):
                        if kp < NKP:
                            unit_s(kp)
                        if kp >= LAG:
                            unit_pv(kp - LAG)
                    S.op("dve", lambda: nc.vector.tensor_tensor(out=accb[j][:], in0=acc[j][:], in1=accp[j][:], op=ALU.add),
                         reads=[b_acc[j], b_accp[j]], writes=[b_accb[j]])
                    S.op("pe", lambda: nc.tensor.matmul(P.ps[0], ones[:], accb[j][:, 0, :], start=True, stop=False),
                         reads=[b_accb[j], b_const], writes=[b_pp[0]], signal=False)
                    S.op("pe", lambda: nc.tensor.matmul(P.ps[0], ones[:], accb[j][:, 1, :], start=False, stop=True),
                         reads=[b_accb[j], b_const], writes=[b_pp[0]])
                    S.op("dve", lambda: nc.vector.reciprocal(out=rinv[j][:], in_=P.ps[0]), reads=[b_pp[0]], writes=[b_rinv[j]])
                    S.op("dve", lambda j=j, qo=qo, s=s, qs=qs: nc.vector.tensor_tensor(out=osb[s][:, qs], in0=P.ps[qo], in1=rinv[j][:], op=ALU.mult),
                         reads=[P.b_ps[qo], b_rinv[j]], writes=[b_osb[s]])
                S.dma("sp", [(oT[h * 128:(h + 1) * 128, :], osb[s][:])], reads=[b_osb[s]], key="osb%d" % s)
            S.barrier()


    sgu_g_d = P.dt("sgu_ln_g", [DEPTH, D], F32, EI)
    sgu_b_d = P.dt("sgu_ln_b", [DEPTH, D], F32, EI)
    sgu_ws_d = P.dt("sgu_ws", [DEPTH, 4, 128, 128], F32, EI)
    sgu_bs_d = P.dt("sgu_bs", [DEPTH, 512], F32, EI)
    dft_cc = P.dt("dft_cc", [2, 512, 512], BF16, EI)
    dft_s = P.dt("dft_s", [2, T, T], BF16, EI)
    sguT = P.dt("sguT", [D, T], BF16)
    Gcs = P.dt("Gcs", [2, T, D], BF16)
    fnT = P.dt("fnT", [D, T], BF16)

    def ln_rows(pes_tiles, h, out, gb, b_h, b_out, b_gb):
        st6, mv, b_small = pes_tiles
        for c in range(4):
            S.op("dve", lambda c=c: nc.vector.bn_stats(out=st6[:, c, :], in_=h[:, c * 512:(c + 1) * 512]), reads=[b_h], writes=[b_small])
        S.op("dve", lambda: nc.vector.bn_aggr(out=mv[:, 0:2], in_=st6[:]), reads=[b_small], writes=[b_small])
        S.op("act", lambda: nc.scalar.activation(out=mv[:, 2:3], in_=mv[:, 1:2], func=AF.Sqrt, bias=eps_t[:, 0:1]), reads=[b_small, b_const], writes=[b_small])
        S.op("dve", lambda: nc.vector.reciprocal(out=mv[:, 2:3], in_=mv[:, 2:3]), reads=[b_small], writes=[b_small])
        S.op("dve", lambda: nc.vector.scalar_tensor_tensor(out=mv[:, 3:4], in0=mv[:, 0:1], scalar=-1.0, in1=mv[:, 2:3], op0=ALU.mult, op1=ALU.mult),
             reads=[b_small], writes=[b_small])
        S.op("act", lambda: nc.scalar.activation(out=h[:], in_=h[:], func=AF.Identity, scale=mv[:, 2:3], bias=mv[:, 3:4]), reads=[b_small], writes=[b_h])
        S.op("dve", lambda: nc.vector.tensor_tensor(out=h[:], in0=h[:], in1=gb[0][:], op=ALU.mult), reads=[b_gb], writes=[b_h])
        S.op("dve", lambda: nc.vector.tensor_tensor(out=out, in0=h[:], in1=gb[1][:], op=ALU.add), reads=[b_h, b_gb], writes=[b_out])

    eps_t = P.sb(es, "eps_t", [128, 1], F32)
    S.op("dve", lambda: nc.vector.memset(eps_t[:], 1e-5), writes=[b_const])

    def phase_sgu(l):
        with ExitStack() as pes:
            gt = P.sb(pes, "sg_g", [128, D], F32)
            bt = P.sb(pes, "sg_b", [128, D], F32)
            bsb = P.sb(pes, "sg_bs", [128, 4, 128], F32)
            wsr = P.sb(pes, "sg_wsr", [128, 4, 128], BF16)
            wsT = P.sb(pes, "sg_wsT", [128, 4, 128], BF16)
            b_w = S.buf("sgw")
            S.dma("sp", [(gt[:], sgu_g_d[l].partition_broadcast(128)), (bt[:], sgu_b_d[l].partition_broadcast(128)),
                         (bsb[:].rearrange("p g q -> p (g q)"), sgu_bs_d[l].partition_broadcast(128))], writes=[b_w], key="sgw")
            S.dma("pool", [(wsr[:], sgu_ws_d[l].rearrange("g p q -> p g q"))], writes=[b_w], key="sgw2")
            pst = P.ps[7].bitcast(BF16)
            for g in range(4):
                S.op("pe", lambda g=g: nc.tensor.transpose(pst[:, g * 128:(g + 1) * 128], wsr[:, g, :], ident[:]),
                     reads=[b_w, b_const], writes=[P.b_ps[7]], signal=(g == 3))
            S.op("dve", lambda: nc.vector.tensor_copy(out=wsT[:].rearrange("q g p -> q (g p)"), in_=pst[:, 0:512]), reads=[P.b_ps[7]], writes=[b_w])
            vt = [P.sb(pes, "sg_v%d" % i, [128, D], BF16) for i in range(2)]
            b_vt = S.bufs_n("sgv", 2)
            hh = [P.sb(pes, "sg_h%d" % i, [128, D], F32) for i in range(2)]
            b_hh = S.bufs_n("sgh", 2)
            vn = [P.sb(pes, "sg_vn%d" % i, [128, D], BF16) for i in range(2)]
            b_vn = S.bufs_n("sgvn", 2)
            ut = [P.sb(pes, "sg_u%d" % i, [128, 16, 512], BF16) for i in range(2)]
            b_ut = S.bufs_n("sgu", 2)
            ot = [P.sb(pes, "sg_o%d" % i, [128, 16, 512], BF16) for i in range(2)]
            b_ot = S.bufs_n("sgo", 2)
            tmp = [P.sb(pes, "sg_t%d" % i, [128, 4, 128], F32) for i in range(2)]
            b_tmp = S.bufs_n("sgt", 2)
            st6 = P.sb(pes, "sg_st6", [128, 4, 6], F32)
            mv = P.sb(pes, "sg_mv", [128, 4], F32)
            b_small = S.buf("sgsmall")
            for tb in range(NTB):
                sb_ = tb % 2
                S.dma("sp", [(ut[sb_][:, a * 4:(a + 1) * 4, :], uT[a * 512:(a + 1) * 512, tb * 512:(tb + 1) * 512].rearrange("(j p) t -> p j t", p=128))
                             for a in range(4)], writes=[b_ut[sb_]], key="sgu%d" % sb_)
                for ci in range(4):
                    tt = tb * 4 + ci
                    s2 = tt % 2
                    S.dma("sp", [(vt[s2][:], vtok[tt * 128:(tt + 1) * 128, :])], writes=[b_vt[s2]], key="sgv%d" % s2)
                    S.op("act", lambda s2=s2: nc.scalar.activation(out=hh[s2][:], in_=vt[s2][:], func=AF.Copy), reads=[b_vt[s2]], writes=[b_hh[s2]])
                    ln_rows((st6, mv, b_small), hh[s2], vn[s2][:], (gt, bt), b_hh[s2], b_vn[s2], b_w)
                    for g in range(4):
                        q = g % 4
                        for ct in range(4):
                            S.op("pe", lambda g=g, ct=ct, q=q, s2=s2: nc.tensor.matmul(P.ps[q][:, ct * 128:(ct + 1) * 128], vn[s2][:, g * 512 + ct * 128:g * 512 + (ct + 1) * 128],
                                                                                 wsT[:, g, :], start=True, stop=True),
                                 reads=[b_vn[s2], b_w], writes=[P.b_ps[q]], signal=(ct == 3))
                        j = g % 2
                        S.op("dve", lambda g=g, q=q, j=j: nc.vector.tensor_tensor(out=tmp[j][:], in0=P.ps[q].rearrange("p (c t) -> p c t", c=4),
                                                                                in1=bsb[:, g:g + 1, :].broadcast_to([128, 4, 128]), op=ALU.add),
                             reads=[P.b_ps[q], b_w], writes=[b_tmp[j]])
                        S.op("dve", lambda g=g, j=j, sb_=sb_, ci=ci: nc.vector.tensor_tensor(out=ot[sb_][:, g * 4:(g + 1) * 4, ci * 128:(ci + 1) * 128], in0=tmp[j][:],
                                                                                         in1=ut[sb_][:, g * 4:(g + 1) * 4, ci * 128:(ci + 1) * 128], op=ALU.mult),
                             reads=[b_tmp[j], b_ut[sb_]], writes=[b_ot[sb_]])
                S.dma("sp", [(sguT[a * 512:(a + 1) * 512, tb * 512:(tb + 1) * 512].rearrange("(j p) t -> p j t", p=128), ot[sb_][:, a * 4:(a + 1) * 4, :])
                             for a in range(4)], reads=[b_ot[sb_]], key="sgo%d" % sb_)
            S.barrier()

    def phase_fnet(l):
        with ExitStack() as pes:
            cc = P.sb(pes, "fn_cc", [128, 2, 4, 512], BF16)
            b_cc = S.buf("fncc")
            S.dma("sp", [(cc[:, i, :, :], dft_cc[i].rearrange("(c p) m -> p c m", p=128)) for i in range(2)], writes=[b_cc], key="fncc")
            ft = [P.sb(pes, "fn_f%d" % i, [128, 16, 512], BF16) for i in range(2)]
            b_ft = S.bufs_n("fnf", 2)
            go = [P.sb(pes, "fn_go%d" % i, [128, 2, D], BF16) for i in range(2)]
            b_go = S.bufs_n("fngo", 2)
            for tb in range(NTB):
                sb_ = tb % 2
                S.dma("sp", [(ft[sb_][:, a * 4:(a + 1) * 4, :], fT[a * 512:(a + 1) * 512, tb * 512:(tb + 1) * 512].rearrange("(j p) t -> p j t", p=128))
                             for a in range(4)], writes=[b_ft[sb_]], key="fnf%d" % sb_)
                for ci in range(4):
                    tt = tb * 4 + ci
                    s2 = tt % 2
                    for g in range(4):
                        for i in range(2):
                            q = (g * 2 + i) % 4
                            for k in range(4):
                                S.op("pe", lambda g=g, i=i, k=k, q=q, ci=ci, sb_=sb_: nc.tensor.matmul(P.ps[q], ft[sb_][:, g * 4 + k, ci * 128:(ci + 1) * 128], cc[:, i, k, :],
                                                                                              start=(k == 0), stop=(k == 3)),
                                     reads=[b_ft[sb_], b_cc], writes=[P.b_ps[q]], signal=(k == 3))
                            evac(g * 2 + i, go[s2][:, i, g * 512:(g + 1) * 512], P.ps[q], [P.b_ps[q]], [b_go[s2]])
                    S.dma("sp", [(Gcs[i][tt * 128:(tt + 1) * 128, :], go[s2][:, i, :]) for i in range(2)], reads=[b_go[s2]], key="fngo%d" % s2)
            S.barrier()
        with ExitStack() as pes:
            Gq = P.sb(pes, "fn_G", [128, 2, NTT, 512], BF16)
            b_G = S.buf("fnG")
            pn = [P.sb(pes, "fn_pn%d" % i, [128, 2, NTT, 512], BF16) for i in range(2)]
            b_pn = S.bufs_n("fnpn", 2)
            yo = [P.sb(pes, "fn_yo%d" % i, [128, 512], BF16) for i in range(4)]
            b_yo = S.bufs_n("fnyo", 4)
            it = 0
            for mq in range(4):
                S.dma("sp", [(Gq[:, i, a * 8:(a + 1) * 8, :], Gcs[i][a * 1024:(a + 1) * 1024, mq * 512:(mq + 1) * 512].rearrange("(st p) m -> p st m", p=128))
                             for i in range(2) for a in range(4)], writes=[b_G], key="fnG")
                for kb in range(NTB):
                    sl = it % 2
                    it += 1
                    S.dma("sp", [(pn[sl][:, i, a * 8:(a + 1) * 8, :], dft_s[i][a * 1024:(a + 1) * 1024, kb * 512:(kb + 1) * 512].rearrange("(st p) k -> p st k", p=128))
                                 for i in range(2) for a in range(4)], writes=[b_pn[sl]], key="fnpn%d" % sl)
                    for mt in range(4):
                        q = mt % 4
                        n = 0
                        for i in range(2):
                            for st_ in range(NTT):
                                S.op("pe", lambda i=i, st_=st_, mt=mt, q=q, sl=sl, n=n: nc.tensor.matmul(P.ps[q], Gq[:, i, st_, mt * 128:(mt + 1) * 128], pn[sl][:, i, st_, :],
                                                                                                start=(n == 0), stop=(n == 2 * NTT - 1)),
                                     reads=[b_G, b_pn[sl]], writes=[P.b_ps[q]], signal=(n == 2 * NTT - 1))
                                n += 1
                        evac(mt, yo[q][:], P.ps[q], [P.b_ps[q]], [b_yo[q]])
                        S.dma("sp", [(fnT[(mq * 4 + mt) * 128:(mq * 4 + mt + 1) * 128, kb * 512:(kb + 1) * 512], yo[q][:])], reads=[b_yo[q]], key="fnyo%d" % q)
            S.barrier()


    w_mla_o = P.dt("w_mla_o", [DEPTH, D, D], F32, EI)
    w_sgu_o = P.dt("w_sgu_o", [DEPTH, D, D], F32, EI)
    w_fnet_o = P.dt("w_fnet_o", [DEPTH, D, D], F32, EI)
    w_out = P.dt("w_out", [DEPTH, D, D], F32, EI)
    w_cq = P.dt("w_cq", [DEPTH, D, D], F32, EI)
    w_ck = P.dt("w_ck", [DEPTH, D, D], F32, EI)
    w_cv = P.dt("w_cv", [DEPTH, D, D], F32, EI)
    w_co = P.dt("w_co", [DEPTH, D, D], F32, EI)
    ln_gb = {k: P.dt(k, [DEPTH, D], F32, EI) for k in ("ln1_g", "ln1_b", "ln2_g", "ln2_b", "ln3_g", "ln3_b")}
    mem_in = P.dt("mem_in", [2 * MEM, D], F32, EI)
    yabc = [P.dt("y_%s" % c, [D, T], BF16) for c in "abc"]
    hpre = P.dt("hpre", [T, D], F32)
    x1 = P.dt("x1", [T, D], F32)
    x2 = P.dt("x2", [T, D], F32)
    x2b = P.dt("x2b", [T, D], BF16)
    qcT = P.dt("qcT", [D, T], BF16)
    kcT = P.dt("kcT", [D, 2 * MEM], BF16)
    vcm = P.dt("vcm", [2 * MEM, D], BF16)
    ocT = P.dt("ocT", [D, T], BF16)

    def load_A(A, b_A, srcs, pes):
        S.dma("sp", [(A[:, a * 4:(a + 1) * 4, :], srcs[0][a * 512:(a + 1) * 512, :].rearrange("(j p) t -> p j t", p=128)) for a in range(4)],
              writes=[b_A], key="ldA")
        if len(srcs) > 1:
            tmp = [P.sb(pes, "ldA_t%d" % i, [128, T], BF16) for i in range(2)]
            b_tmp = S.bufs_n("ldAt", 2)
            n = 0
            for j in range(16):
                for src in srcs[1:]:
                    i = n % 2
                    n += 1
                    S.dma("sp", [(tmp[i][:], src[j * 128:(j + 1) * 128, :])], writes=[b_tmp[i]], key="ldAt%d" % i)
                    S.op("dve", lambda i=i, j=j: nc.vector.tensor_tensor(out=A[:, j, :], in0=A[:, j, :], in1=tmp[i][:], op=ALU.add),
                         reads=[b_tmp[i]], writes=[b_A])

    def phase_branch_proj(l):
        for bi, (src, W) in enumerate([(oT, w_mla_o), (sguT, w_sgu_o), (fnT, w_fnet_o)]):
            with ExitStack() as pes:
                A = P.sb(pes, "bpA", [128, 16, T], BF16)
                b_A = S.buf("bpA")
                load_A(A, b_A, [src], pes)
                G = Gemm(P, S, pes, 16)
                gt = [P.sb(pes, "bp_g%d" % i, [128, T], BF16) for i in range(2)]
                b_gt = S.bufs_n("bpg", 2)
                yo = [P.sb(pes, "bp_y%d" % i, [128, T], BF16) for i in range(2)]
                b_yo = S.bufs_n("bpy", 2)
                dst = yabc[bi]

                def epi(q, m, n0, tb, width, bi=bi, dst=dst):
                    j = n0 % 2
                    if tb == 0:
                        S.dma("sp", [(gt[j][:], gT[bi * D + n0 * 128:bi * D + (n0 + 1) * 128, :])], writes=[b_gt[j]], key="bpg%d" % j)
                    S.op("dve", lambda: nc.vector.tensor_tensor(out=yo[j][:, tb * 512:(tb + 1) * 512], in0=P.ps[q], in1=gt[j][:, tb * 512:(tb + 1) * 512], op=ALU.mult),
                         reads=[P.b_ps[q], b_gt[j]], writes=[b_yo[j]])
                    if tb == NTB - 1:
                        S.dma("sp", [(dst[n0 * 128:(n0 + 1) * 128, :], yo[j][:])], reads=[b_yo[j]], key="bpy%d" % j)
                G.run(A, b_A, W[l], D, "fm", epi)
                S.barrier()

    def phase_proj_ln(l, srcs, W, xres, gname, bname, dst, dst_b=None):
        with ExitStack() as pes:
            A = P.sb(pes, "plA", [128, 16, T], BF16)
            b_A = S.buf("plA")
            load_A(A, b_A, srcs, pes)
            G = Gemm(P, S, pes, 16)
            xt = [P.sb(pes, "pl_x%d" % i, [128, 512], F32) for i in range(4)]
            b_xt = S.bufs_n("plx", 4)
            ho = [P.sb(pes, "pl_h%d" % i, [128, 512], F32) for i in range(4)]
            b_ho = S.bufs_n("plh", 4)

            def epi(q, m, n0, tt, width):
                j = tt % 4
                cs = slice(n0, n0 + 512)
                S.dma("sp", [(xt[j][:], xres[tt * 128:(tt + 1) * 128, cs])], writes=[b_xt[j]], key="plx%d" % j)
                S.op("dve", lambda: nc.vector.scalar_tensor_tensor(out=ho[j][:], in0=xt[j][:], scalar=ALPHA, in1=P.ps[q], op0=ALU.mult, op1=ALU.add),
                     reads=[b_xt[j], P.b_ps[q]], writes=[b_ho[j]])
                S.dma("sp", [(hpre[tt * 128:(tt + 1) * 128, cs], ho[j][:])], reads=[b_ho[j]], key="plh%d" % j)
            G.run(A, b_A, W[l], D, "tm", epi)
            S.barrier()
        phase_ln(l, hpre, gname, bname, dst, dst_b)

    def phase_ln(l, src, gname, bname, dst, dst_b=None):
        with ExitStack() as pes:
            gt = P.sb(pes, "ln_g", [128, D], F32)
            bt = P.sb(pes, "ln_b", [128, D], F32)
            b_gb = S.buf("lngb")
            S.dma("sp", [(gt[:], ln_gb[gname][l].partition_broadcast(128)), (bt[:], ln_gb[bname][l].partition_broadcast(128))], writes=[b_gb], key="lngb")
            hh = [P.sb(pes, "ln_h%d" % i, [128, D], F32) for i in range(3)]
            b_hh = S.bufs_n("lnh", 3)
            oo = [P.sb(pes, "ln_o%d" % i, [128, D], F32) for i in range(3)]
            b_oo = S.bufs_n("lno", 3)
            ob = [P.sb(pes, "ln_ob%d" % i, [128, D], BF16) for i in range(2)]
            b_ob = S.bufs_n("lnob", 2)
            st6 = P.sb(pes, "ln_st6", [128, 4, 6], F32)
            mv = P.sb(pes, "ln_mv", [128, 4], F32)
            b_small = S.buf("lnsmall")
            for tt in range(NTT):
                j = tt % 3
                rows = slice(tt * 128, (tt + 1) * 128)
                S.dma("sp", [(hh[j][:], src[rows, :])], writes=[b_hh[j]], key="lnh%d" % j)
                ln_rows((st6, mv, b_small), hh[j], oo[j][:], (gt, bt), b_hh[j], b_oo[j], b_gb)
                S.dma("sp", [(dst[rows, :], oo[j][:])], reads=[b_oo[j]], key="lno%d" % j)
                if dst_b is not None:
                    jb = tt % 2
                    S.op("act", lambda j=j, jb=jb: nc.scalar.activation(out=ob[jb][:], in_=oo[j][:], func=AF.Copy), reads=[b_oo[j]], writes=[b_ob[jb]])
                    S.dma("sp", [(dst_b[rows, :], ob[jb][:])], reads=[b_ob[jb]], key="lnob%d" % jb)
            S.barrier()

    XSCALE = 512 ** -0.5

    def phase_cross_qkv(l):
        with ExitStack() as pes:
            xT = P.sb(pes, "cxT", [128, 16, T], BF16)
            b_xT = S.buf("cxT")
            G = Gemm(P, S, pes, 16)
            build_xT(xT, b_xT, G.wp, G.b_wp, x1, T)
            yo = [P.sb(pes, "cq_y%d" % i, [128, T], BF16) for i in range(2)]
            b_yo = S.bufs_n("cqy", 2)

            def epi(q, m, n0, tb, width):
                j = n0 % 2
                evac(tb, yo[j][:, tb * 512:(tb + 1) * 512], P.ps[q], [P.b_ps[q]], [b_yo[j]])
                if tb == NTB - 1:
                    S.dma("sp", [(qcT[n0 * 128:(n0 + 1) * 128, :], yo[j][:])], reads=[b_yo[j]], key="cqy%d" % j)
            G.run(xT, b_xT, w_cq[l], D, "fm", epi)
            S.barrier()
        with ExitStack() as pes:
            mT = P.sb(pes, "cmT", [128, 16, 2 * MEM], BF16)
            b_mT = S.buf("cmT")
            G = Gemm(P, S, pes, 16)
            build_xT(mT, b_mT, G.wp, G.b_wp, mem_in, 2 * MEM)
            ko = [P.sb(pes, "ck_y%d" % i, [128, 512], BF16) for i in range(4)]
            b_ko = S.bufs_n("cky", 4)

            def epi_k(q, m, n0, tb, width):
                j = n0 % 4
                evac(n0, ko[j][:], P.ps[q], [P.b_ps[q]], [b_ko[j]])
                S.dma("sp", [(kcT[n0 * 128:(n0 + 1) * 128, :], ko[j][:])], reads=[b_ko[j]], key="cky%d" % j)
            G.run(mT, b_mT, w_ck[l], D, "fm", epi_k, Tc=2 * MEM)

            def epi_v(q, m, n0, tt, width):
                j = tt % 4
                evac(tt, ko[j][:], P.ps[q], [P.b_ps[q]], [b_ko[j]])
                S.dma("sp", [(vcm[tt * 128:(tt + 1) * 128, n0:n0 + 512], ko[j][:])], reads=[b_ko[j]], key="cky%d" % j)
            G.run(mT, b_mT, w_cv[l], D, "tm", epi_v, Tc=2 * MEM)
            S.barrier()

    def phase_cross_attn(l):
        with ExitStack() as pes:
            kc = P.sb(pes, "xa_k", [128, 16, 2 * MEM], BF16)
            vc = P.sb(pes, "xa_v", [128, 4, D], BF16)
            b_kv = S.buf("xakv")
            S.dma("sp", [(kc[:, a * 4:(a + 1) * 4, :], kcT[a * 512:(a + 1) * 512, :].rearrange("(j p) m -> p j m", p=128)) for a in range(4)]
                  + [(vc[:], vcm.rearrange("(a p) d -> p a d", p=128))], writes=[b_kv], key="xakv")
            qh = [P.sb(pes, "xa_q%d" % i, [128, 4, T], BF16) for i in range(2)]
            b_qh = S.bufs_n("xaq", 2)
            sq = P.sb(pes, "xa_sq", [128, 4, T], BF16)
            b_sq = S.buf("xasq")
            mx = P.sb(pes, "xa_mx", [128, 32], F32)
            b_mx = S.buf("xamx")
            Pt = [P.sb(pes, "xa_P%d" % i, [128, 2, 512], BF16) for i in range(2)]
            b_pt = S.bufs_n("xaP", 2)
            rinv = [P.sb(pes, "xa_r%d" % i, [128, 512], F32) for i in range(2)]
            b_rinv = S.bufs_n("xar", 2)
            osb = [P.sb(pes, "xa_o%d" % i, [128, 4, T], BF16) for i in range(2)]
            b_osb = S.bufs_n("xao", 2)
            b_pp = S.bufs_n("xapp", 2)

            def load_q(h):
                s = h % 2
                S.dma("sp", [(qh[s][:], qcT[h * 512:(h + 1) * 512, :].rearrange("(j p) t -> p j t", p=128))], writes=[b_qh[s]], key="xaq%d" % s)
            load_q(0)
            for h in range(4):
                s = h % 2
                if h + 1 < 4:
                    load_q(h + 1)
                S.op("act", lambda: nc.scalar.activation(out=sq[:], in_=qh[s][:], func=AF.Square), reads=[b_qh[s]], writes=[b_sq])
                for c in range(NTB):
                    q = 6 + c % 2
                    for k in range(4):
                        S.op("pe", lambda c=c, k=k, q=q: nc.tensor.matmul(P.ps[q], ones[:], sq[:, k, c * 512:(c + 1) * 512], start=(k == 0), stop=(k == 3)),
                             reads=[b_sq, b_const], writes=[P.b_ps[q]], signal=(k == 3))
                    S.op("dve", lambda c=c, q=q: nc.vector.reduce_max(out=mx[:, c:c + 1], in_=P.ps[q], axis=AX.X), reads=[P.b_ps[q]], writes=[b_mx])
                S.op("act", lambda: nc.scalar.activation(out=sq[:, :, 0:512], in_=kc[:, h * 4:(h + 1) * 4, :], func=AF.Square), reads=[b_kv, b_sq], writes=[b_sq])
                for k in range(4):
                    S.op("pe", lambda k=k: nc.tensor.matmul(P.ps[6], ones[:], sq[:, k, 0:512], start=(k == 0), stop=(k == 3)),
                         reads=[b_sq, b_const], writes=[P.b_ps[6]], signal=(k == 3))
                S.op("dve", lambda: nc.vector.reduce_max(out=mx[:, 17:18], in_=P.ps[6], axis=AX.X), reads=[P.b_ps[6]], writes=[b_mx])
                S.op("dve", lambda: nc.vector.reduce_max(out=mx[:, 16:17], in_=mx[:, 0:8], axis=AX.X), reads=[b_mx], writes=[b_mx])
                S.op("dve", lambda: nc.vector.tensor_tensor(out=mx[:, 18:19], in0=mx[:, 16:17], in1=mx[:, 17:18], op=ALU.mult), reads=[b_mx], writes=[b_mx])
                S.op("act", lambda: nc.scalar.activation(out=mx[:, 19:20], in_=mx[:, 18:19], func=AF.Sqrt, scale=XSCALE * XSCALE), reads=[b_mx], writes=[b_mx])
                S.op("dve", lambda: nc.vector.tensor_scalar(out=mx[:, 20:21], in0=mx[:, 19:20], scalar1=-1.0, scalar2=None, op0=ALU.mult), reads=[b_mx], writes=[b_mx])
                for tb in range(NTB):
                    seq = tb // 4
                    j = tb % 2
                    ts_ = slice(tb * 512, (tb + 1) * 512)
                    for mt in range(2):
                        for c in range(4):
                            S.op("pe", lambda mt=mt, c=c: nc.tensor.matmul(P.pst[:, j * 2 + mt, :], kc[:, h * 4 + c, seq * MEM + mt * 128:seq * MEM + (mt + 1) * 128],
                                                                           qh[s][:, c, ts_], start=(c == 0), stop=(c == 3)),
                                 reads=[b_kv, b_qh[s]], writes=[b_pp[j]], signal=(c == 3 and mt == 1))
                    S.op("act", lambda: nc.scalar.activation(out=Pt[j][:], in_=P.pst[:, j * 2:j * 2 + 2, :], func=AF.Exp, scale=XSCALE, bias=mx[:, 20:21]),
                         reads=[b_pp[j], b_mx], writes=[b_pt[j]])
                    for mt in range(2):
                        S.op("pe", lambda mt=mt: nc.tensor.matmul(P.ps[4 + j], ones[:], Pt[j][:, mt, :], start=(mt == 0), stop=(mt == 1)),
                             reads=[b_pt[j], b_const], writes=[P.b_ps[4 + j]], signal=(mt == 1))
                    S.op("dve", lambda: nc.vector.reciprocal(out=rinv[j][:], in_=P.ps[4 + j]), reads=[P.b_ps[4 + j]], writes=[b_rinv[j]])
                    for dt_ in range(4):
                        q = 6 + dt_ % 2
                        for mt in range(2):
                            S.op("pe", lambda mt=mt, dt_=dt_, q=q: nc.tensor.matmul(P.ps[q], vc[:, seq * 2 + mt, h * 512 + dt_ * 128:h * 512 + (dt_ + 1) * 128],
                                                                                Pt[j][:, mt, :], start=(mt == 0), stop=(mt == 1)),
                                 reads=[b_kv, b_pt[j]], writes=[P.b_ps[q]], signal=(mt == 1))
                        S.op("dve", lambda dt_=dt_, q=q: nc.vector.tensor_tensor(out=osb[s][:, dt_, ts_], in0=P.ps[q], in1=rinv[j][:], op=ALU.mult),
                             reads=[P.b_ps[q], b_rinv[j]], writes=[b_osb[s]])
                S.dma("sp", [(ocT[h * 512:(h + 1) * 512, :].rearrange("(j p) t -> p j t", p=128), osb[s][:])], reads=[b_osb[s]], key="xao%d" % s)
            S.barrier()


    w_router = P.dt("w_router", [DEPTH, D, NEXP], F32, EI)
    b_router = P.dt("b_router", [DEPTH, NEXP], F32, EI)
    w_gu = P.dt("w_gu", [DEPTH, NEXP, D, 2 * D], F32, EI)
    b_gu = P.dt("b_gu", [DEPTH, NEXP, 128, 32], F32, EI)
    w_down = P.dt("w_down", [DEPTH, NEXP, D, D], F32, EI)
    b_down = P.dt("b_down", [DEPTH, NEXP, D], F32, EI)
    utri_d = P.dt("utri", [128, 128], BF16, EI)
    ecap_d = P.dt("ecap", [128, NEXP], F32, EI)
    tokid_d = P.dt("tokid", [128, NTT], I32, EI)
    slot_tok = P.dt("slot_tok", [NEXP * CAP, 1], I32)
    tok_slot = P.dt("tok_slot", [T, 4], I32)
    tok_prob = P.dt("tok_prob", [T, 4], F32)
    yslot = P.dt("yslot", [NEXP * CAP, D], BF16)
    x_l1 = P.dt("x_l1", [T, D], F32)
    y_out = P.dt("y_out", [T, D], F32, "ExternalOutput")

    def phase_router(l):
        with ExitStack() as pes:
            xT = P.sb(pes, "rxT", [128, 16, T], BF16)
            b_xT = S.buf("rxT")
            G = Gemm(P, S, pes, 16)
            build_xT(xT, b_xT, G.wp, G.b_wp, x2, T)
            wr = P.sb(pes, "r_wr", [128, 16, NEXP], BF16)
            br = P.sb(pes, "r_br", [128, NEXP], F32)
            utri = P.sb(pes, "r_utri", [128, 128], BF16)
            ecap = P.sb(pes, "r_ecap", [128, NEXP], F32)
            tokid = P.sb(pes, "r_tokid", [128, NTT], I32)
            sent = P.sb(pes, "r_sent", [128, NEXP * CAP // 128], I32)
            b_rc = S.buf("rconst")
            S.dma("pool", [(wr[:], w_router[l].rearrange("(c p) e -> p c e", p=128))], writes=[b_rc], key="rc1")
            S.dma("sp", [(br[:], b_router[l].partition_broadcast(128)), (utri[:], utri_d[:, :]), (ecap[:], ecap_d[:, :]), (tokid[:], tokid_d[:, :])],
                  writes=[b_rc], key="rc2")
            S.op("dve", lambda: nc.vector.memset(sent[:], T), writes=[b_rc])
            b_slottok = S.buf("slottok")
            S.dma("sp", [(slot_tok.rearrange("(p a) o -> p (a o)", p=128), sent[:])], reads=[b_rc], writes=[b_slottok], key="rc3")
            carry = P.sb(pes, "r_carry", [128, NEXP], F32)
            b_carry = S.buf("rcarry")
            S.op("dve", lambda: nc.vector.memset(carry[:], 0.0), writes=[b_carry])
            NB = 2
            lg = [P.sb(pes, "r_lg%d" % i, [128, NEXP], F32) for i in range(NB)]
            t8 = [P.sb(pes, "r_t8%d" % i, [128, 16], F32) for i in range(NB)]
            mk = [P.sb(pes, "r_mk%d" % i, [128, NEXP], BF16) for i in range(NB)]
            se = [P.sb(pes, "r_se%d" % i, [128, NEXP], F32) for i in range(NB)]
            junk = [P.sb(pes, "r_junk%d" % i, [128, NEXP], F32) for i in range(NB)]
            pk = [P.sb(pes, "r_pk%d" % i, [128, 8], F32) for i in range(NB)]
            slf = [P.sb(pes, "r_slf%d" % i, [128, 4], F32) for i in range(NB)]
            sli = [P.sb(pes, "r_sli%d" % i, [128, 4], I32) for i in range(NB)]
            b_t = S.bufs_n("rt", NB)
            for tt in range(NTT):
                i = tt % NB
                q = tt % 2
                bt_ = b_t[i]
                for k in range(16):
                    S.op("pe", lambda k=k, q=q: nc.tensor.matmul(P.ps[q][:, 0:NEXP], xT[:, k, tt * 128:(tt + 1) * 128], wr[:, k, :], start=(k == 0), stop=(k == 15)),
                         reads=[b_xT, b_rc], writes=[P.b_ps[q]], signal=(k == 15))
                S.op("dve", lambda: nc.vector.tensor_tensor(out=lg[i][:], in0=P.ps[q][:, 0:NEXP], in1=br[:], op=ALU.add), reads=[P.b_ps[q], b_rc], writes=[bt_])
                S.op("dve", lambda: nc.vector.max(out=t8[i][:, 0:8], in_=lg[i][:]), reads=[bt_], writes=[bt_])
                S.op("dve", lambda: nc.vector.tensor_scalar(out=mk[i][:], in0=lg[i][:], scalar1=t8[i][:, 3:4], scalar2=None, op0=ALU.is_ge), reads=[bt_], writes=[bt_])
                S.op("dve", lambda: nc.vector.tensor_scalar(out=t8[i][:, 8:9], in0=t8[i][:, 0:1], scalar1=-1.0, scalar2=None, op0=ALU.mult), reads=[bt_], writes=[bt_])
                S.op("act", lambda: nc.scalar.activation(out=pk[i][:, 0:4], in_=t8[i][:, 0:4], func=AF.Exp, bias=t8[i][:, 8:9], accum_out=pk[i][:, 4:5]),
                     reads=[bt_], writes=[bt_])
                S.op("dve", lambda: nc.vector.reciprocal(out=pk[i][:, 5:6], in_=pk[i][:, 4:5]), reads=[bt_], writes=[bt_])
                S.op("dve", lambda: nc.vector.tensor_scalar(out=pk[i][:, 0:4], in0=pk[i][:, 0:4], scalar1=pk[i][:, 5:6], scalar2=None, op0=ALU.mult), reads=[bt_], writes=[bt_])
                qr, qt = 2 + tt % 2, 4 + tt % 2
                S.op("pe", lambda: nc.tensor.matmul(P.ps[qr][:, 0:NEXP], utri[:], mk[i][:], start=True, stop=True), reads=[bt_, b_rc], writes=[P.b_ps[qr]])
                S.op("pe", lambda: nc.tensor.matmul(P.ps[qt][:, 0:NEXP], ones[:], mk[i][:], start=True, stop=True), reads=[bt_, b_const], writes=[P.b_ps[qt]])
                S.op("dve", lambda: nc.vector.tensor_tensor(out=se[i][:], in0=P.ps[qr][:, 0:NEXP], in1=carry[:], op=ALU.add), reads=[P.b_ps[qr], b_carry], writes=[bt_])
                S.op("dve", lambda: nc.vector.tensor_tensor(out=carry[:], in0=P.ps[qt][:, 0:NEXP], in1=carry[:], op=ALU.add), reads=[P.b_ps[qt]], writes=[b_carry])
                S.op("dve", lambda: nc.vector.scalar_tensor_tensor(out=se[i][:], in0=se[i][:], scalar=float(CAP - 1), in1=ecap[:], op0=ALU.min, op1=ALU.add),
                     reads=[b_rc], writes=[bt_])
                for k in range(4):
                    S.op("dve", lambda k=k: nc.vector.scalar_tensor_tensor(out=junk[i][:], in0=lg[i][:], scalar=t8[i][:, k:k + 1], in1=se[i][:], op0=ALU.is_equal, op1=ALU.mult,
                                                                         accum_out=slf[i][:, k:k + 1]), reads=[bt_], writes=[bt_])
                S.op("dve", lambda: nc.vector.tensor_copy(out=sli[i][:], in_=slf[i][:]), reads=[bt_], writes=[bt_])
                rows = slice(tt * 128, (tt + 1) * 128)
                S.dma("sp", [(tok_slot[rows, :], sli[i][:]), (tok_prob[rows, :], pk[i][:, 0:4])], reads=[bt_], key="rst%d" % i)
                for k in range(4):
                    S.dma_raw("pool", lambda k=k: nc.gpsimd.indirect_dma_start(out=slot_tok, out_offset=bass.IndirectOffsetOnAxis(ap=sli[i][:, k:k + 1], axis=0),
                                                                              in_=tokid[:, tt:tt + 1], in_offset=None),
                              reads=[bt_, b_rc], writes=[b_slottok], key="rsc")
            S.barrier()

    bc_reg = nc.gpsimd.alloc_register("bcreg")
    nc.gpsimd.reg_mov(bc_reg, T - 1)

    def phase_experts(l):
        with ExitStack() as pes:
            NH = CAP // 2
            idx = [P.sb(pes, "e_idx%d" % i, [128, CAP // 128], I32) for i in range(2)]
            b_idx = S.bufs_n("eidx", 2)
            NXG = 6
            xg = [P.sb(pes, "e_xg%d" % i, [128, D], BF16) for i in range(NXG)]
            b_xg = S.bufs_n("exg", NXG)
            for i in range(NXG):
                S.op("dve", lambda i=i: nc.vector.memset(xg[i][:], 0.0), writes=[b_xg[i]])
            xgT = P.sb(pes, "e_xgT", [128, 16, CAP], BF16)
            b_xgT = S.buf("exgT")
            actT = P.sb(pes, "e_actT", [128, 16, CAP], BF16)
            b_actT = S.buf("eactT")
            wg = [P.sb(pes, "e_wg%d" % i, [128, 2, 16, 512], BF16) for i in range(2)]
            b_wg = S.bufs_n("ewg", 2)
            wd = [P.sb(pes, "e_wd%d" % i, [128, 16, 512], BF16) for i in range(2)]
            b_wd = S.bufs_n("ewd", 2)
            bgu = [P.sb(pes, "e_bgu%d" % i, [128, 32], F32) for i in range(2)]
            bdn = [P.sb(pes, "e_bdn%d" % i, [128, D], BF16) for i in range(2)]
            b_bias = S.bufs_n("ebias", 2)
            for i in range(2):
                S.op("dve", lambda i=i: nc.vector.memset(bdn[i][:], 0.0), writes=[b_bias[i]])
            gc = [P.sb(pes, "e_gc%d" % i, [128, NH], F32) for i in range(2)]
            sg = [P.sb(pes, "e_sg%d" % i, [128, NH], F32) for i in range(2)]
            uc = [P.sb(pes, "e_uc%d" % i, [128, NH], F32) for i in range(2)]
            b_ep = S.bufs_n("eep", 2)
            yo = [P.sb(pes, "e_yo%d" % i, [128, 512], BF16) for i in range(4)]
            b_yo = S.bufs_n("eyo", 4)
            wcount = [0, 0]
            for e in range(NEXP):
                eb = e % 2
                S.dma("sp", [(bgu[eb][:], b_gu[l, e]), (idx[eb][:], slot_tok[e * CAP:(e + 1) * CAP, :].rearrange("(p a) o -> p (a o)", p=128))],
                      writes=[b_bias[eb], b_idx[eb]], key="eb%d" % eb)
                S.dma("pool", [(bdn[eb][0:1, :], b_down[l, e:e + 1, :])], writes=[b_bias[eb]], key="ebd%d" % eb)
                for st_ in range(CAP // 128):
                    gi = st_ % NXG
                    S.dma_raw("pool", lambda st_=st_, gi=gi: nc.gpsimd.indirect_dma_start(
                        out=xg[gi][:], out_offset=None, in_=x2b, in_offset=bass.IndirectOffsetOnAxis(ap=idx[eb][:, st_:st_ + 1], axis=0),
                        bounds_check=bc_reg, oob_is_err=False), reads=[b_idx[eb]], writes=[b_xg[gi]], key="eg%d" % gi)
                    for half in range(2):
                        q = 6 + half
                        pst = P.ps[q].bitcast(BF16)
                        for j in range(8):
                            c = half * 8 + j
                            S.op("pe", lambda c=c, j=j, pst=pst, gi=gi: nc.tensor.transpose(pst[:, j * 128:(j + 1) * 128], xg[gi][:, c * 128:(c + 1) * 128], ident[:]),
                                 reads=[b_xg[gi], b_const], writes=[P.b_ps[q]], signal=(j == 7))
                        evac(half, xgT[:, half * 8:(half + 1) * 8, st_ * 128:(st_ + 1) * 128], pst.rearrange("p (j t) -> p j t", j=8), [P.b_ps[q]], [b_xgT])
                for jp in range(4):
                    sl = wcount[0] % 2
                    wcount[0] += 1
                    S.dma("pool", [(wg[sl][:, 0, :, :], w_gu[l, e][:, jp * 512:(jp + 1) * 512].rearrange("(c p) n -> p c n", p=128)),
                                   (wg[sl][:, 1, :, :], w_gu[l, e][:, D + jp * 512:D + (jp + 1) * 512].rearrange("(c p) n -> p c n", p=128))],
                          writes=[b_wg[sl]], key="ewg%d" % sl)
                    for sub in range(4):
                        j = jp * 4 + sub
                        for hf in range(2):
                            pi = (sub * 2 + hf) % 2
                            qg, qu = pi * 2, pi * 2 + 1
                            for gu, qq in ((0, qg), (1, qu)):
                                for k in range(16):
                                    S.op("pe", lambda gu=gu, qq=qq, k=k, sub=sub, hf=hf, sl=sl: nc.tensor.matmul(
                                        P.ps[qq][:, 0:NH], wg[sl][:, gu, k, sub * 128:(sub + 1) * 128], xgT[:, k, hf * NH:(hf + 1) * NH], start=(k == 0), stop=(k == 15)),
                                        reads=[b_wg[sl], b_xgT], writes=[P.b_ps[qq]], signal=(k == 15))
                            S.op("dve", lambda pi=pi, qg=qg, j=j: nc.vector.tensor_scalar(out=gc[pi][:], in0=P.ps[qg][:, 0:NH], scalar1=bgu[eb][:, j:j + 1], scalar2=7.0, op0=ALU.add, op1=ALU.min),
                                 reads=[P.b_ps[qg], b_bias[eb]], writes=[b_ep[pi]])
                            S.op("act", lambda pi=pi: nc.scalar.activation(out=sg[pi][:], in_=gc[pi][:], func=AF.Sigmoid, scale=1.702), reads=[b_ep[pi]], writes=[b_ep[pi]])
                            S.op("dve", lambda pi=pi, qu=qu, j=j: nc.vector.tensor_scalar(out=uc[pi][:], in0=P.ps[qu][:, 0:NH], scalar1=bgu[eb][:, 16 + j:17 + j], scalar2=7.0, op0=ALU.add, op1=ALU.min),
                                 reads=[P.b_ps[qu], b_bias[eb]], writes=[b_ep[pi]])
                            S.op("dve", lambda pi=pi: nc.vector.tensor_scalar(out=uc[pi][:], in0=uc[pi][:], scalar1=-7.0, scalar2=1.0, op0=ALU.max, op1=ALU.add), reads=[b_ep[pi]], writes=[b_ep[pi]])
                            S.op("dve", lambda pi=pi: nc.vector.tensor_tensor(out=gc[pi][:], in0=gc[pi][:], in1=sg[pi][:], op=ALU.mult), reads=[b_ep[pi]], writes=[b_ep[pi]])
                            S.op("dve", lambda pi=pi, j=j, hf=hf: nc.vector.tensor_tensor(out=actT[:, j, hf * NH:(hf + 1) * NH], in0=gc[pi][:], in1=uc[pi][:], op=ALU.mult),
                                 reads=[b_ep[pi]], writes=[b_actT])
                for dp in range(4):
                    sl = wcount[1] % 2
                    wcount[1] += 1
                    S.dma("pool", [(wd[sl][:], w_down[l, e][:, dp * 512:(dp + 1) * 512].rearrange("(c p) n -> p c n", p=128))], writes=[b_wd[sl]], key="ewd%d" % sl)
                    for st_ in range(CAP // 128):
                        q = 4 + st_ % 2
                        for k in range(16):
                            S.op("pe", lambda k=k, q=q, st_=st_, sl=sl: nc.tensor.matmul(P.ps[q], actT[:, k, st_ * 128:(st_ + 1) * 128], wd[sl][:, k, :], start=(k == 0), stop=False),
                                 reads=[b_actT, b_wd[sl]], writes=[P.b_ps[q]], signal=False)
                        S.op("pe", lambda q=q, dp=dp: nc.tensor.matmul(P.ps[q], ones[:], bdn[eb][:, dp * 512:(dp + 1) * 512], start=False, stop=True),
                             reads=[b_bias[eb], b_const], writes=[P.b_ps[q]])
                        yi = (dp * 6 + st_) % 4
                        evac(st_, yo[yi][:], P.ps[q], [P.b_ps[q]], [b_yo[yi]])
                        S.dma("sp", [(yslot[e * CAP:(e + 1) * CAP, dp * 512:(dp + 1) * 512].rearrange("(p a) d -> a p d", p=128)[st_], yo[yi][:])], reads=[b_yo[yi]], key="eyo%d" % yi)
            S.barrier()

    def phase_combine(l, dst):
        with ExitStack() as pes:
            gt = P.sb(pes, "c_g", [128, D], F32)
            bt = P.sb(pes, "c_b", [128, D], F32)
            b_gb = S.buf("cgb")
            S.dma("sp", [(gt[:], ln_gb["ln3_g"][l].partition_broadcast(128)), (bt[:], ln_gb["ln3_b"][l].partition_broadcast(128))], writes=[b_gb], key="cgb")
            NB = 2
            sl = [P.sb(pes, "c_sl%d" % i, [128, 4], I32) for i in range(NB)]
            pk = [P.sb(pes, "c_pk%d" % i, [128, 4], F32) for i in range(NB)]
            b_sp = S.bufs_n("csp", NB)
            yr = [P.sb(pes, "c_yr%d" % i, [128, D], BF16) for i in range(4)]
            b_yr = S.bufs_n("cyr", 4)
            hh = [P.sb(pes, "c_h%d" % i, [128, D], F32) for i in range(NB)]
            b_hh = S.bufs_n("chh", NB)
            oo = [P.sb(pes, "c_o%d" % i, [128, D], F32) for i in range(NB)]
            b_oo = S.bufs_n("coo", NB)
            st6 = P.sb(pes, "c_st6", [128, 4, 6], F32)
            mv = P.sb(pes, "c_mv", [128, 4], F32)
            b_small = S.buf("csmall")
            for tt in range(NTT):
                i = tt % NB
                rows = slice(tt * 128, (tt + 1) * 128)
                S.dma("sp", [(sl[i][:], tok_slot[rows, :]), (pk[i][:], tok_prob[rows, :])], writes=[b_sp[i]], key="csp%d" % i)
                S.dma("sp", [(oo[i][:], x2[rows, :])], writes=[b_oo[i]], key="cx%d" % i)
                S.op("act", lambda: nc.scalar.activation(out=hh[i][:], in_=oo[i][:], func=AF.Copy, scale=ALPHA), reads=[b_oo[i]], writes=[b_hh[i]])
                for k in range(4):
                    yi = (tt * 4 + k) % 4
                    S.dma_raw("pool", lambda k=k, yi=yi: nc.gpsimd.indirect_dma_start(
                        out=yr[yi][:], out_offset=None, in_=yslot, in_offset=bass.IndirectOffsetOnAxis(ap=sl[i][:, k:k + 1], axis=0)),
                        reads=[b_sp[i]], writes=[b_yr[yi]], key="cg%d" % yi)
                    S.op("dve", lambda k=k, yi=yi: nc.vector.scalar_tensor_tensor(out=hh[i][:], in0=yr[yi][:], scalar=pk[i][:, k:k + 1], in1=hh[i][:], op0=ALU.mult, op1=ALU.add),
                         reads=[b_yr[yi], b_sp[i]], writes=[b_hh[i]])
                ln_rows((st6, mv, b_small), hh[i], oo[i][:], (gt, bt), b_hh[i], b_oo[i], b_gb)
                S.dma("sp", [(dst[rows, :], oo[i][:])], reads=[b_oo[i]], key="co%d" % i)
            S.barrier()

    for l in layers:
        x_src = x_in if l == 0 else x_l1
        if want("zgemm"):
            phase_zgemm(l, x_src)
        if want("mla_prep"):
            phase_mla_prep(l)
        if want("attn"):
            phase_attn(l)
        if want("sgu"):
            phase_sgu(l)
        if want("fnet"):
            phase_fnet(l)
        if want("proj1"):
            phase_branch_proj(l)
            phase_proj_ln(l, yabc, w_out, x_src, "ln1_g", "ln1_b", x1)
        if want("cross"):
            phase_cross_qkv(l)
            phase_cross_attn(l)
            phase_proj_ln(l, [ocT], w_co, x1, "ln2_g", "ln2_b", x2, x2b)
        if want("moe"):
            phase_router(l)
            phase_experts(l)
            phase_combine(l, y_out if l == DEPTH - 1 else x_l1)

    S.finish()
    P.ninstr = S.ninstr
    return P


_PROG = {}


def _bf16():
    import ml_dtypes
    return ml_dtypes.bfloat16


def _seq_tables(segs):
    BF = _bf16()
    inv = 1.0 / (10000.0 ** (np.arange(0, 64, 2, dtype=np.float32) / 64))
    pos = np.concatenate([np.arange(L, dtype=np.float32) for L in segs])
    ang = pos[:, None] * inv[None, :]
    cos, sin = np.cos(ang).T.astype(np.float32), np.sin(ang).T.astype(np.float32)
    rope_cs = np.stack([np.concatenate([cos, cos], 0), np.concatenate([-sin, sin], 0)]).astype(np.float32)
    seg_id = np.concatenate([np.full(L, i) for i, L in enumerate(segs)])
    mask = np.zeros((128, NTT * NTB), np.float32)
    for kt in range(NTT):
        for qc in range(NTB):
            if seg_id[kt * 128] != seg_id[qc * 512]:
                mask[:, kt * NTB + qc] = -30000.0
    dft = np.zeros((2, T, T), np.float32)
    o = 0
    for L in segs:
        a = (np.outer(np.arange(L), np.arange(L)) % L).astype(np.float64) * (2 * np.pi / L)
        nrm = 1.0 / np.sqrt(L * 512)
        dft[0, o:o + L, o:o + L] = np.cos(a) * nrm
        dft[1, o:o + L, o:o + L] = -np.sin(a) * nrm
        o += L
    return rope_cs, mask, dft.astype(BF)


def _const_inputs():
    BF = _bf16()
    ac = (np.outer(np.arange(512), np.arange(512)) % 512).astype(np.float64) * (2 * np.pi / 512)
    return {
        "ident": np.eye(128, dtype=np.float32).astype(BF),
        "dft_cc": np.stack([np.cos(ac), np.sin(ac)]).astype(np.float32).astype(BF),
        "utri": np.triu(np.ones((128, 128), np.float32), 1).astype(BF),
        "ecap": np.tile((np.arange(NEXP) * CAP).astype(np.float32)[None, :], (128, 1)),
        "tokid": (np.arange(NTT)[None, :] * 128 + np.arange(128)[:, None]).astype(np.int32),
    }


def _pc(v, c):
    return np.ascontiguousarray(v.reshape(v.shape[0], c, 128).transpose(0, 2, 1))


def make_in_maps(inputs):
    f = {k: np.asarray(v) for k, v in inputs.items()}
    shared = {k: np.ascontiguousarray(f[k], dtype=np.float32) for k in (
        "w_in", "w_uq", "w_ukv", "w_mla_o", "sgu_ln_g", "sgu_ln_b", "sgu_ws", "w_sgu_o", "w_fnet_o", "w_out",
        "ln1_g", "ln1_b", "w_cq", "w_ck", "w_cv", "w_co", "ln2_g", "ln2_b", "w_router", "b_router", "w_gu",
        "w_down", "b_down", "ln3_g", "ln3_b")}
    shared["b_gate"] = np.ascontiguousarray(f["b_gate"], dtype=np.float32).reshape(DEPTH, 3 * D, 1)
    shared["mla_q_norm"] = _pc(f["mla_q_norm"].astype(np.float32), 4)
    shared["mla_kv_norm"] = _pc(f["mla_kv_norm"].astype(np.float32), 4)
    shared["sgu_bs"] = np.ascontiguousarray(f["sgu_bs"], dtype=np.float32).reshape(DEPTH, 512)
    shared["b_gu"] = np.ascontiguousarray(f["b_gu"].astype(np.float32).reshape(DEPTH, NEXP, 32, 128).transpose(0, 1, 3, 2))
    shared.update(_const_inputs())
    tab_pair = _seq_tables([2048, 2048])
    tab_samp = _seq_tables([4096])
    xp, xs, mp, ms = f["x_prompt"], f["x_sample"], f["mem_prompt"], f["mem_sample"]
    groups = []
    for c in range(4):
        groups.append(("pair", np.concatenate([xp[2 * c], xp[2 * c + 1]], 0), np.concatenate([mp[2 * c], mp[2 * c + 1]], 0)))
    for c in range(2):
        groups.append(("samp", xs[c], np.concatenate([ms[c], ms[c]], 0)))
    groups.append(groups[0])
    groups.append(groups[4])
    in_maps = []
    for kind, x, mem in groups:
        rope_cs, mask, dft = tab_pair if kind == "pair" else tab_samp
        m = dict(shared)
        m.update({"x_in": np.ascontiguousarray(x, dtype=np.float32), "mem_in": np.ascontiguousarray(mem, dtype=np.float32),
                  "rope_cs": rope_cs, "attn_mask": mask, "dft_s": dft})
        in_maps.append(m)
    return in_maps


def kernel(**inputs):
    if "P" not in _PROG:
        _PROG["P"] = build_program()
    P = _PROG["P"]
    in_maps = make_in_maps(inputs)
    res = run_bass_kernel_spmd(P.nc, in_maps, core_ids=list(range(8)))
    outs = [np.asarray(r["y_out"], dtype=np.float32) for r in res.results]
    y_prompt = np.stack([outs[c // 2][(c % 2) * 2048:(c % 2 + 1) * 2048] for c in range(8)], 0)
    y_sample = np.stack([outs[4], outs[5]], 0)
    return (y_prompt, y_sample)
```
